# Optimizing a Trainium2 kernel written in Bass

```python
import math
import jax
import jax.numpy as jnp
from jax import lax
import numpy as np


D_MODEL = 1024
BATCH = 4
SEQ = 8192
DEPTH = 1

NORM_EPS = 1e-6
NEG_INF = -1e30
Q_BLOCK = 128

DA_HEADS = 8
DA_DIM = 64
DA_VDIM = 2 * DA_DIM
DA_SUBLN_EPS = 1e-5

NSA_HEADS = 8
NSA_GROUPS = 2
NSA_HPG = NSA_HEADS // NSA_GROUPS
NSA_DIM = 64
CMP_BLOCK = 32
CMP_STRIDE = 16
CMP_HIDDEN = 128
SEL_BLOCK = 64
SEL_TOPK = 16
WINDOW = 512
FORCE_BONUS = 1e4

N_BRANCH = 2
N_ALIBI_HEADS = DA_HEADS + NSA_HEADS

N_EXPERTS = 64
MOE_TOPK = 8
N_EXPERT_GROUPS = 8
TOPK_EXPERT_GROUPS = 4
EXPERT_HIDDEN = 256
SHARED_HIDDEN = 256
ROUTED_SCALE = 2.5
MOE_BLOCK = 128

IN_SIZES = (
    DA_HEADS * 2 * DA_DIM,
    DA_HEADS * 2 * DA_DIM,
    DA_HEADS * DA_VDIM,
    NSA_HEADS * NSA_DIM,
    NSA_GROUPS * NSA_DIM,
    NSA_GROUPS * NSA_DIM,
    NSA_GROUPS * NSA_DIM,
    NSA_GROUPS * NSA_DIM,
    NSA_GROUPS * NSA_DIM,
    NSA_GROUPS * NSA_DIM,
    NSA_HEADS * 3,
    N_BRANCH * D_MODEL,
)
IN_COLS = 6424

kernel_name = 'hybrid_diffattn_nsa_moe_block'


def _lambda_init(layer):
    return 0.8 - 0.6 * math.exp(-0.3 * layer)


def _alibi_slopes():
    i = jnp.arange(1, N_ALIBI_HEADS + 1, dtype=jnp.float32)
    return 2.0 ** (-8.0 * i / N_ALIBI_HEADS)


def _rms(x, eps):
    xf = x.astype(jnp.float32)
    return xf * lax.rsqrt(jnp.mean(xf * xf, axis=-1, keepdims=True) + eps)


def _modulate(x, w, shift, scale):
    y = _rms(x, NORM_EPS) * w.astype(jnp.float32)
    return (y * (1.0 + scale[:, None, :]) + shift[:, None, :]).astype(x.dtype)


def _swiglu(x, w_gate, w_up, w_down):
    return (jax.nn.silu(x @ w_gate) * (x @ w_up)) @ w_down


def diff_attention(q, k, v, lam, slopes):
    B, H, _, S, _ = q.shape
    key_pos = jnp.arange(S)
    m = slopes[None, :, None, None, None]

    def one_block(i):
        q0 = i * Q_BLOCK
        qb = lax.dynamic_slice_in_dim(q, q0, Q_BLOCK, axis=3)
        dist = (q0 + jnp.arange(Q_BLOCK))[:, None] - key_pos[None, :]
        s = jnp.einsum('bhcqd,bhckd->bhcqk', qb, k) - m * dist.astype(jnp.float32)
        p = jax.nn.softmax(jnp.where(dist >= 0, s, NEG_INF), axis=-1)
        a = p[:, :, 0] - lam * p[:, :, 1]
        return jnp.einsum('bhqk,bhkd->bhqd', a, v)

    o = lax.map(one_block, jnp.arange(S // Q_BLOCK))
    return o.transpose(1, 2, 0, 3, 4).reshape(B, H, S, -1)


def compress_blocks(k, pe, w1, w2):
    B, G, S, Dh = k.shape
    n_cmp = (S - CMP_BLOCK) // CMP_STRIDE + 1
    idx = jnp.arange(n_cmp)[:, None] * CMP_STRIDE + jnp.arange(CMP_BLOCK)[None, :]
    blk = k[:, :, idx, :] + pe.astype(jnp.float32)
    flat = blk.reshape(B, G, n_cmp, CMP_BLOCK * Dh)
    return jax.nn.gelu(flat @ w1.astype(jnp.float32)) @ w2.astype(jnp.float32)


def _cmp_to_sel(n_cmp, n_sel):
    c0 = jnp.arange(n_cmp)[:, None] * CMP_STRIDE
    s0 = jnp.arange(n_sel)[None, :] * SEL_BLOCK
    ov = jnp.minimum(c0 + CMP_BLOCK, s0 + SEL_BLOCK) - jnp.maximum(c0, s0)
    return jnp.clip(ov, 0).astype(jnp.float32) / CMP_BLOCK


def nsa_attention(q, kc, vc, ks, vs, kw, vw, gates, slopes):
    B, G, Hg, S, Dh = q.shape
    n_cmp = kc.shape[2]
    n_sel = S // SEL_BLOCK
    n_top = min(SEL_TOPK, n_sel)
    cmp_pos = jnp.arange(n_cmp) * CMP_STRIDE + CMP_BLOCK - 1
    cmp_to_sel = _cmp_to_sel(n_cmp, n_sel)
    ks_blk = ks.reshape(B, G, n_sel, SEL_BLOCK, Dh)
    vs_blk = vs.reshape(B, G, n_sel, SEL_BLOCK, Dh)
    pad = ((0, 0), (0, 0), (WINDOW, 0), (0, 0))
    kw_pad = jnp.pad(kw, pad)
    vw_pad = jnp.pad(vw, pad)
    b_ix = jnp.arange(B)[:, None, None, None]
    g_ix = jnp.arange(G)[None, :, None, None]
    blk_ids = jnp.arange(n_sel)
    m = slopes[None, :, :, None, None]

    def one_block(i):
        q0 = i * Q_BLOCK
        t = q0 + jnp.arange(Q_BLOCK)
        qb = lax.dynamic_slice_in_dim(q, q0, Q_BLOCK, axis=3)
        gb = lax.dynamic_slice_in_dim(gates, q0, Q_BLOCK, axis=3)
        d_c = t[:, None] - cmp_pos[None, :]
        ok_c = d_c >= 0
        s_c = jnp.einsum('bghqd,bgcd->bghqc', qb, kc) - m * d_c.astype(jnp.float32)
        p_c = jax.nn.softmax(jnp.where(ok_c, s_c, NEG_INF), axis=-1) * ok_c
        o_c = jnp.einsum('bghqc,bgcd->bghqd', p_c, vc)
        imp = jnp.einsum('bghqc,cn->bgqn', p_c, cmp_to_sel)
        cur = t // SEL_BLOCK
        forced = (blk_ids[None, :] == 0) | (blk_ids[None, :] == cur[:, None]) | (blk_ids[None, :] == cur[:, None] - 1)
        causal_blk = blk_ids[None, :] * SEL_BLOCK <= t[:, None]
        sel_score = jnp.where(causal_blk, imp + FORCE_BONUS * forced, NEG_INF)
        _, sel = lax.top_k(sel_score, n_top)
        k_sel = ks_blk[b_ix, g_ix, sel]
        v_sel = vs_blk[b_ix, g_ix, sel]
        tok = sel[..., None] * SEL_BLOCK + jnp.arange(SEL_BLOCK)
        d_s = (t[:, None, None] - tok)[:, :, None]
        s_s = jnp.einsum('bghqd,bgqnld->bghqnl', qb, k_sel) - m[..., None] * d_s.astype(jnp.float32)
        s_s = jnp.where(d_s >= 0, s_s, NEG_INF).reshape(B, G, Hg, Q_BLOCK, n_top * SEL_BLOCK)
        p_s = jax.nn.softmax(s_s, axis=-1).reshape(B, G, Hg, Q_BLOCK, n_top, SEL_BLOCK)
        o_s = jnp.einsum('bghqnl,bgqnld->bghqd', p_s, v_sel)
        k_w = lax.dynamic_slice_in_dim(kw_pad, q0, Q_BLOCK + WINDOW, axis=2)
        v_w = lax.dynamic_slice_in_dim(vw_pad, q0, Q_BLOCK + WINDOW, axis=2)
        key_pos = q0 - WINDOW + jnp.arange(Q_BLOCK + WINDOW)
        d_w = t[:, None] - key_pos[None, :]
        ok_w = (d_w >= 0) & (d_w < WINDOW) & (key_pos[None, :] >= 0)
        s_w = jnp.einsum('bghqd,bgkd->bghqk', qb, k_w) - m * d_w.astype(jnp.float32)
        p_w = jax.nn.softmax(jnp.where(ok_w, s_w, NEG_INF), axis=-1)
        o_w = jnp.einsum('bghqk,bgkd->bghqd', p_w, v_w)
        return gb[..., 0:1] * o_c + gb[..., 1:2] * o_s + gb[..., 2:3] * o_w

    o = lax.map(one_block, jnp.arange(S // Q_BLOCK))
    return o.transpose(1, 0, 4, 2, 3, 5).reshape(B, S, G * Hg * Dh)


def hybrid_mixer(h, w_in, lq1, lk1, lq2, lk2, subln_w, ck_pe, ck_w1, ck_w2, cv_pe, cv_w1, cv_w2,
                 w_da_out, w_nsa_out, w_o, lam_init):
    B, S, D = h.shape
    f32 = jnp.float32
    proj = h @ w_in
    offsets = np.cumsum(IN_SIZES)[:-1].tolist()
    (da_q, da_k, da_v, nsa_q, c_k, c_v, s_k, s_v, w_k, w_v, nsa_g, merge_g) = jnp.split(proj, offsets, axis=-1)
    slopes = _alibi_slopes()
    slopes_a = slopes[0::2]
    slopes_b = slopes[1::2]

    qa = da_q.reshape(B, S, DA_HEADS, 2, DA_DIM).transpose(0, 2, 3, 1, 4).astype(f32) * (DA_DIM ** -0.5)
    ka = da_k.reshape(B, S, DA_HEADS, 2, DA_DIM).transpose(0, 2, 3, 1, 4).astype(f32)
    va = da_v.reshape(B, S, DA_HEADS, DA_VDIM).transpose(0, 2, 1, 3).astype(f32)
    lam = (jnp.exp(jnp.sum(lq1.astype(f32) * lk1.astype(f32)))
           - jnp.exp(jnp.sum(lq2.astype(f32) * lk2.astype(f32))) + lam_init)
    oa = diff_attention(qa, ka, va, lam, slopes_a)
    oa = _rms(oa, DA_SUBLN_EPS) * subln_w.astype(f32) * (1.0 - lam_init)
    oa = oa.transpose(0, 2, 1, 3).reshape(B, S, DA_HEADS * DA_VDIM).astype(h.dtype)

    def kv_groups(t):
        return t.reshape(B, S, NSA_GROUPS, NSA_DIM).transpose(0, 2, 1, 3).astype(f32)
    qb = nsa_q.reshape(B, S, NSA_GROUPS, NSA_HPG, NSA_DIM).transpose(0, 2, 3, 1, 4).astype(f32) * (NSA_DIM ** -0.5)
    kc = compress_blocks(kv_groups(c_k), ck_pe, ck_w1, ck_w2)
    vc = compress_blocks(kv_groups(c_v), cv_pe, cv_w1, cv_w2)
    gates = jax.nn.sigmoid(nsa_g.astype(f32)).reshape(B, S, NSA_GROUPS, NSA_HPG, 3).transpose(0, 2, 3, 1, 4)
    ob = nsa_attention(qb, kc, vc, kv_groups(s_k), kv_groups(s_v), kv_groups(w_k), kv_groups(w_v),
                       gates, slopes_b.reshape(NSA_GROUPS, NSA_HPG)).astype(h.dtype)

    ya = oa @ w_da_out
    yb = ob @ w_nsa_out
    g = jax.nn.sigmoid(merge_g.astype(f32)).reshape(B, S, N_BRANCH, D)
    merged = (g[:, :, 0] * ya + g[:, :, 1] * yb).astype(h.dtype)
    return merged @ w_o


def _routed_experts(xf, top_e, top_w, w_gate, w_up, w_down):
    N, D = xf.shape
    K = top_e.shape[1]
    E = w_gate.shape[0]
    flat_e = top_e.reshape(-1)
    order = jnp.argsort(flat_e)
    sorted_e = flat_e[order]
    counts = jnp.bincount(flat_e, length=E)
    padded = ((counts + MOE_BLOCK - 1) // MOE_BLOCK) * MOE_BLOCK
    start = jnp.cumsum(counts) - counts
    pend = jnp.cumsum(padded)
    pstart = pend - padded
    dest = pstart[sorted_e] + jnp.arange(N * K) - start[sorted_e]
    n_rows = ((N * K + E * (MOE_BLOCK - 1) + MOE_BLOCK - 1) // MOE_BLOCK) * MOE_BLOCK
    n_blk = n_rows // MOE_BLOCK
    row_tok = jnp.full((n_rows,), N, jnp.int32).at[dest].set((order // K).astype(jnp.int32))
    row_w = jnp.zeros((n_rows,), jnp.float32).at[dest].set(top_w.reshape(-1)[order])
    blk_e = jnp.clip(jnp.searchsorted(pend, jnp.arange(n_blk) * MOE_BLOCK, side='right'), 0, E - 1)
    x_pad = jnp.concatenate([xf, jnp.zeros((1, D), xf.dtype)], axis=0)

    def one_group(args):
        tok, e = args
        xb = x_pad[tok]
        return _swiglu(xb, w_gate[e], w_up[e], w_down[e])

    y = lax.map(one_group, (row_tok.reshape(n_blk, MOE_BLOCK), blk_e))
    y = y.reshape(n_rows, D).astype(jnp.float32) * row_w[:, None]
    return jax.ops.segment_sum(y, row_tok, num_segments=N + 1)[:N]


def moe_ffn(h, router_w, router_b, w_gate, w_up, w_down, sw_gate, sw_up, sw_down):
    B, S, D = h.shape
    N = B * S
    xf = h.reshape(N, D)
    scores = jax.nn.sigmoid((xf @ router_w).astype(jnp.float32))
    biased = scores + router_b.astype(jnp.float32)
    grp = biased.reshape(N, N_EXPERT_GROUPS, N_EXPERTS // N_EXPERT_GROUPS)
    grp_score = jnp.sum(lax.top_k(grp, 2)[0], axis=-1)
    _, top_g = lax.top_k(grp_score, TOPK_EXPERT_GROUPS)
    g_mask = jnp.sum(jax.nn.one_hot(top_g, N_EXPERT_GROUPS, dtype=jnp.float32), axis=1) > 0
    e_mask = jnp.repeat(g_mask, N_EXPERTS // N_EXPERT_GROUPS, axis=1)
    _, top_e = lax.top_k(jnp.where(e_mask, biased, NEG_INF), MOE_TOPK)
    top_w = jnp.take_along_axis(scores, top_e, axis=1)
    top_w = top_w / jnp.sum(top_w, axis=-1, keepdims=True) * ROUTED_SCALE
    routed = _routed_experts(xf, top_e, top_w, w_gate, w_up, w_down)
    shared = _swiglu(xf, sw_gate, sw_up, sw_down).astype(jnp.float32)
    return (routed + shared).astype(h.dtype).reshape(B, S, D)


def setup_inputs(seed: int = 0) -> dict:
    key = jax.random.key(seed)
    ks = jax.random.split(key, 30)
    f32 = jnp.float32

    def nrm(k, shape, scale):
        return jax.random.normal(k, shape, f32) * scale

    D = D_MODEL
    L = DEPTH
    return {
        'x': nrm(ks[0], (BATCH, SEQ, D), 1.0),
        'c': nrm(ks[1], (BATCH, D), 1.0),
        'ada_w': nrm(ks[2], (L, D, 6 * D), 0.5 * D ** -0.5),
        'ada_b': nrm(ks[3], (L, 6 * D), 0.02),
        'norm1_w': 1.0 + nrm(ks[4], (L, D), 0.02),
        'w_in': nrm(ks[5], (L, D, IN_COLS), D ** -0.5),
        'da_lq1': nrm(ks[6], (L, DA_DIM), 0.1),
        'da_lk1': nrm(ks[7], (L, DA_DIM), 0.1),
        'da_lq2': nrm(ks[8], (L, DA_DIM), 0.1),
        'da_lk2': nrm(ks[9], (L, DA_DIM), 0.1),
        'da_subln_w': 1.0 + nrm(ks[10], (L, DA_VDIM), 0.02),
        'cmp_k_pe': nrm(ks[11], (L, CMP_BLOCK, NSA_DIM), 0.02),
        'cmp_k_w1': nrm(ks[12], (L, CMP_BLOCK * NSA_DIM, CMP_HIDDEN), (CMP_BLOCK * NSA_DIM) ** -0.5),
        'cmp_k_w2': nrm(ks[13], (L, CMP_HIDDEN, NSA_DIM), CMP_HIDDEN ** -0.5),
        'cmp_v_pe': nrm(ks[14], (L, CMP_BLOCK, NSA_DIM), 0.02),
        'cmp_v_w1': nrm(ks[15], (L, CMP_BLOCK * NSA_DIM, CMP_HIDDEN), (CMP_BLOCK * NSA_DIM) ** -0.5),
        'cmp_v_w2': nrm(ks[16], (L, CMP_HIDDEN, NSA_DIM), CMP_HIDDEN ** -0.5),
        'w_da_out': nrm(ks[17], (L, DA_HEADS * DA_VDIM, D), (DA_HEADS * DA_VDIM) ** -0.5),
        'w_nsa_out': nrm(ks[18], (L, NSA_HEADS * NSA_DIM, D), (NSA_HEADS * NSA_DIM) ** -0.5),
        'w_o': nrm(ks[19], (L, D, D), D ** -0.5),
        'norm2_w': 1.0 + nrm(ks[20], (L, D), 0.02),
        'router_w': nrm(ks[21], (L, D, N_EXPERTS), D ** -0.5),
        'router_b': nrm(ks[22], (L, N_EXPERTS), 0.01),
        'exp_w_gate': nrm(ks[23], (L, N_EXPERTS, D, EXPERT_HIDDEN), D ** -0.5),
        'exp_w_up': nrm(ks[24], (L, N_EXPERTS, D, EXPERT_HIDDEN), D ** -0.5),
        'exp_w_down': nrm(ks[25], (L, N_EXPERTS, EXPERT_HIDDEN, D), EXPERT_HIDDEN ** -0.5),
        'sh_w_gate': nrm(ks[26], (L, D, SHARED_HIDDEN), D ** -0.5),
        'sh_w_up': nrm(ks[27], (L, D, SHARED_HIDDEN), D ** -0.5),
        'sh_w_down': nrm(ks[28], (L, SHARED_HIDDEN, D), SHARED_HIDDEN ** -0.5),
        'final_norm_w': 1.0 + nrm(ks[29], (D,), 0.02),
    }


def reference(x, c, ada_w, ada_b, norm1_w, w_in, da_lq1, da_lk1, da_lq2, da_lk2, da_subln_w,
              cmp_k_pe, cmp_k_w1, cmp_k_w2, cmp_v_pe, cmp_v_w1, cmp_v_w2, w_da_out, w_nsa_out, w_o,
              norm2_w, router_w, router_b, exp_w_gate, exp_w_up, exp_w_down,
              sh_w_gate, sh_w_up, sh_w_down, final_norm_w):
    f32 = jnp.float32
    c_act = jax.nn.silu(c.astype(f32))
    for l in range(DEPTH):
        mod = c_act @ ada_w[l].astype(f32) + ada_b[l].astype(f32)
        sh1, sc1, g1, sh2, sc2, g2 = jnp.split(mod, 6, axis=-1)
        h = _modulate(x, norm1_w[l], sh1, sc1)
        mix = hybrid_mixer(h, w_in[l], da_lq1[l], da_lk1[l], da_lq2[l], da_lk2[l], da_subln_w[l],
                           cmp_k_pe[l], cmp_k_w1[l], cmp_k_w2[l], cmp_v_pe[l], cmp_v_w1[l], cmp_v_w2[l],
                           w_da_out[l], w_nsa_out[l], w_o[l], _lambda_init(l))
        x = (x + g1[:, None, :] * mix).astype(x.dtype)
        h = _modulate(x, norm2_w[l], sh2, sc2)
        ffn = moe_ffn(h, router_w[l], router_b[l], exp_w_gate[l], exp_w_up[l], exp_w_down[l],
                      sh_w_gate[l], sh_w_up[l], sh_w_down[l])
        x = (x + g2[:, None, :] * ffn).astype(x.dtype)
    return (_rms(x, NORM_EPS) * final_norm_w.astype(f32)).astype(x.dtype)
```

```python
import contextlib
import os
import numpy as np
import ml_dtypes
import concourse.bass as bass
import concourse.mybir as mybir
from concourse.bass_utils import run_bass_kernel_spmd

F32 = mybir.dt.float32
BF16 = mybir.dt.bfloat16
AF = mybir.ActivationFunctionType
ALU = mybir.AluOpType
AX = mybir.AxisListType

S = 8192
D = 1024
NQ = 4096
NEG = -30000.0
ENGS = ["pe", "act", "dve", "pool", "sp"]


class Tok:
    __slots__ = ("sem", "val")

    def __init__(self, sem, val):
        self.sem = sem
        self.val = val


class Buf:
    __slots__ = ("w", "r")

    def __init__(self):
        self.w = None
        self.r = {}


class Rot:
    def __init__(self, items):
        self.items = list(items)
        self.i = 0

    def next(self):
        v = self.items[self.i]
        self.i = (self.i + 1) % len(self.items)
        return v


class Prog:
    def __init__(self, nc, stack, n_dma_sems=(32, 16)):
        self.nc = nc
        self.ops = {e: [] for e in ENGS}
        self.cnt = {e: 0 for e in ENGS}
        self.esem = {}
        for e in ["pe", "act", "dve", "pool"]:
            self.esem[e] = stack.enter_context(nc.semaphore("S_" + e))
        self.dpool = {}
        for e, n in zip(["sp", "pool"], n_dma_sems):
            sems = [stack.enter_context(nc.semaphore(f"D_{e}{i}")) for i in range(n)]
            self.dpool[e] = {"sems": sems, "uses": [0] * n, "next": 0, "last": [None] * n}
        self.waited = {e: {} for e in ENGS}
        self.nops = 0

    def buf(self):
        return Buf()

    def bufs(self, n):
        return [Buf() for _ in range(n)]

    def _need(self, eng, tok, waits):
        if tok is None:
            return
        if eng == "pe" and tok.sem is self.esem["pe"]:
            return
        key = id(tok.sem)
        if self.waited[eng].get(key, 0) >= tok.val:
            return
        cur = waits.get(key)
        if cur is None or cur[1] < tok.val:
            waits[key] = (tok.sem, tok.val)

    def op(self, eng, fn, reads=(), writes=(), dma=False):
        waits = {}
        for b in reads:
            self._need(eng, b.w, waits)
        for b in writes:
            self._need(eng, b.w, waits)
            for t in b.r.values():
                self._need(eng, t, waits)
        if dma:
            pool = self.dpool[eng]
            j = pool["next"]
            pool["next"] = (j + 1) % len(pool["sems"])
            self._need(eng, pool["last"][j], waits)
            pool["uses"][j] += 1
            tok = Tok(pool["sems"][j], 16 * pool["uses"][j])
            pool["last"][j] = tok
            inc = 16
        else:
            self.cnt[eng] += 1
            tok = Tok(self.esem[eng], self.cnt[eng])
            inc = 1
        for key, (sem, val) in waits.items():
            self.waited[eng][key] = val
        self.ops[eng].append((list(waits.values()), fn, tok, inc))
        self.nops += 1
        for b in reads:
            b.r[id(tok.sem)] = tok
        for b in writes:
            b.w = tok
            b.r = {}
        return tok

    def wait_all(self, eng, toks):
        waits = {}
        for t in toks:
            self._need(eng, t, waits)
        for key, (sem, val) in waits.items():
            self.waited[eng][key] = val
        if waits:
            self.ops[eng].append((list(waits.values()), None, None, 0))

    def barrier(self):
        toks = [Tok(self.esem[f], self.cnt[f]) for f in ["pe", "act", "dve", "pool"] if self.cnt[f] > 0]
        for pool in self.dpool.values():
            toks += [t for t in pool["last"] if t is not None]
        for e in ENGS:
            self.wait_all(e, toks)

    def flush(self, block):
        def mk(ename):
            ops = self.ops[ename]
            self.ops[ename] = []

            def body(e):
                for waits, fn, tok, inc in ops:
                    for sem, val in waits:
                        e.wait_ge(sem, val)
                    if fn is not None:
                        fn(e).then_inc(tok.sem, inc)
            return body
        block.tensor(mk("pe"))
        block.scalar(mk("act"))
        block.vector(mk("dve"))
        block.gpsimd(mk("pool"))
        block.sync(mk("sp"))

    def mm(self, out, lhsT, rhs, start, stop, R=(), W=()):
        return self.op("pe", lambda e: e.matmul(out, lhsT=lhsT, rhs=rhs, start=start, stop=stop), R, W)

    def tr(self, out, in_, ident, R=(), W=()):
        return self.op("pe", lambda e: e.transpose(out=out, in_=in_, identity=ident), R, W)

    def act(self, out, in_, func, R=(), W=(), bias=None, scale=None, accum_out=None):
        kw = {}
        if bias is not None:
            kw["bias"] = bias
        if scale is not None:
            kw["scale"] = scale
        if accum_out is not None:
            kw["accum_out"] = accum_out
        return self.op("act", lambda e: e.activation(out=out, in_=in_, func=func, **kw), R, W)

    def tt(self, out, in0, in1, op, R=(), W=(), eng="dve"):
        return self.op(eng, lambda e: e.tensor_tensor(out=out, in0=in0, in1=in1, op=op), R, W)

    def ts(self, out, in0, s1, s2, op0, op1=None, R=(), W=(), eng="dve"):
        if op1 is None:
            return self.op(eng, lambda e: e.tensor_scalar(out=out, in0=in0, scalar1=s1, scalar2=None, op0=op0), R, W)
        return self.op(eng, lambda e: e.tensor_scalar(out=out, in0=in0, scalar1=s1, scalar2=s2, op0=op0, op1=op1), R, W)

    def stt(self, out, in0, scalar, in1, op0, op1, R=(), W=()):
        return self.op("dve", lambda e: e.scalar_tensor_tensor(out=out, in0=in0, scalar=scalar, in1=in1, op0=op0, op1=op1), R, W)

    def copy(self, out, in_, R=(), W=(), eng="dve"):
        return self.op(eng, lambda e: e.tensor_copy(out=out, in_=in_), R, W)

    def memset(self, ap, val, W=(), eng="pool"):
        return self.op(eng, lambda e: e.memset(ap, val), (), W)

    def dma(self, out, in_, R=(), W=(), eng="sp"):
        return self.op(eng, lambda e: e.dma_start(out=out, in_=in_), R, W, dma=True)


class Ctx:
    pass


def _slopes():
    i = np.arange(1, 17, dtype=np.float64)
    s = 2.0 ** (-8.0 * i / 16.0)
    return s[0::2], s[1::2]


SL_A, SL_B = _slopes()
SKIP_THR = 48.0


def first_tile(j, m):
    t0 = 0
    while t0 < 8 * j and m * (1024 * j - (128 * t0 + 127)) > SKIP_THR:
        t0 += 1
    return t0

C_DAQ, C_DAK, C_DAV, C_NQ = 0, 1024, 2048, 3072
C_CK, C_CV, C_SK, C_SV, C_WK, C_WV = 3584, 3712, 3840, 3968, 4096, 4224
C_NG, C_MG = 4352, 4376


def build_nc(stop_after=None, debug=False):
    nc = bass.Bass("TRN2", target_bir_lowering=False)
    K = Ctx()
    K.nc = nc
    K.debug = debug

    def din(name, shape, dt=F32):
        return nc.dram_tensor(name, list(shape), dt, kind="ExternalInput").ap()

    def dscr(name, shape, dt=BF16):
        kind = "ExternalOutput" if debug else "Internal"
        return nc.dram_tensor(name, list(shape), dt, kind=kind).ap()

    I = {}
    I["xn"] = din("xn", [S, D]); I["xo"] = din("xo", [NQ, D]); I["cT"] = din("cT", [128, 8])
    I["ada_w"] = din("ada_w", [D, 6 * D]); I["ada_bT"] = din("ada_bT", [128, 48]); I["ada_b"] = din("ada_b", [1, 6 * D])
    I["n1wT"] = din("n1wT", [128, 8]); I["n2wT"] = din("n2wT", [128, 8])
    I["w_in"] = din("w_in", [D, 6424])
    for nm in ["lq1", "lk1", "lq2", "lk2"]:
        I[nm] = din(nm, [1, 64])
    I["subln"] = din("subln", [1, 128])
    I["ck_peT"] = din("ck_peT", [64, 32]); I["ck_w1"] = din("ck_w1", [2048, 128]); I["ck_w2"] = din("ck_w2", [128, 64])
    I["cv_peT"] = din("cv_peT", [64, 32]); I["cv_w1"] = din("cv_w1", [2048, 128]); I["cv_w2"] = din("cv_w2", [128, 64])
    I["w_da_out"] = din("w_da_out", [1024, D]); I["w_nsa_out"] = din("w_nsa_out", [512, D]); I["w_o"] = din("w_o", [D, D])
    I["router_w"] = din("router_w", [D, 64]); I["router_b"] = din("router_b", [1, 64])
    I["eg"] = din("eg", [64, D, 256]); I["eu"] = din("eu", [64, D, 256]); I["ed"] = din("ed", [64, 256, D])
    I["sg"] = din("sg", [D, 256]); I["su"] = din("su", [D, 256]); I["sd"] = din("sd", [256, D])
    I["fnw"] = din("fnw", [1, D])
    I["refda"] = din("refda", [8, NQ], BF16); I["refn"] = din("refn", [8, NQ], BF16); I["kposT"] = din("kposT", [128, 64]); I["cposT"] = din("cposT", [128, 4])
    I["dmask"] = din("dmask", [128, 8, 512], BF16); I["wmask"] = din("wmask", [128, 12, 512], BF16)
    I["cmask"] = din("cmask", [128, 3, 512], BF16); I["addmask"] = din("addmask", [128, 32, 128], BF16)
    I["wind"] = din("wind", [32, S], BF16); I["c2s"] = din("c2s", [128, 4, 128], BF16)
    out_d = nc.dram_tensor("out", [NQ, D], F32, kind="ExternalOutput").ap()
    K.I = I

    Sc = {}
    Sc["ktda"] = dscr("s_ktda", [8, 128, S]); Sc["vda"] = dscr("s_vda", [S, 1024])
    Sc["nfm"] = dscr("s_nfm", [4, 128, S]); Sc["ntm"] = dscr("s_ntm", [S, 256])
    Sc["qtda"] = dscr("s_qtda", [8, 128, NQ]); Sc["qtn"] = dscr("s_qtn", [4, 128, NQ]); Sc["mg"] = dscr("s_mg", [16, 128, NQ], F32)
    Sc["oat"] = dscr("s_oat", [8, 128, NQ]); Sc["obt"] = dscr("s_obt", [4, 128, NQ])
    Sc["x1"] = dscr("s_x1", [NQ, D], F32); Sc["h2t"] = dscr("s_h2t", [8, 128, NQ])
    K.Sc = Sc
    if debug:
        K.dbg_kc = [dscr(f"dbg_kc{g}", [64, 512]) for g in range(2)]
        K.dbg_vc = [dscr(f"dbg_vc{g}", [128, 4, 64]) for g in range(2)]
        K.dbg_mod = dscr("dbg_mod", [128, 48], F32)
        K.dbg_bc = dscr("dbg_bc", [128, 3, D], F32)
        K.dbg_gates = dscr("dbg_gates", [128, 32, 24], F32)
        K.dbg_wr = dscr("dbg_wr", [128, 32, 64], F32)

    with contextlib.ExitStack() as st:
        P = Prog(nc, st)
        K.P = P
        block = st.enter_context(nc.Block())
        K.block = block
        ps = st.enter_context(nc.psum_tensor("ps", [128, 8, 512], F32))
        K.ps = ps
        K.pb = P.bufs(8)

        def sb(name, shape, dt=F32, stack=st):
            return stack.enter_context(nc.sbuf_tensor(name, list(shape), dt))
        K.sb = sb

        K.ident = sb("ident", [128, 128]); K.b_ident = P.buf()
        K.identb = sb("identb", [128, 128], BF16)
        K.modT = sb("modT", [128, 48]); K.b_modT = P.buf()
        K.a1 = sb("a1", [128, 8]); K.a2 = sb("a2", [128, 8]); K.b_a = P.buf()
        K.g1bc = sb("g1bc", [128, D]); K.g2bc = sb("g2bc", [128, D]); K.fnwbc = sb("fnwbc", [128, D]); K.b_gbc = P.buf()
        K.nlam = sb("nlam", [128, 1]); K.b_lam = P.buf()
        K.swbc = sb("swbc", [128, 128]); K.b_sw = P.buf()
        K.gates = sb("gates", [128, 32, 24]); K.b_gates = P.buf()
        K.Wr = sb("Wr", [128, 32, 64]); K.b_Wr = P.buf()
        K.rbbc = sb("rbbc", [128, 64]); K.b_rb = P.buf()
        K.kposT = sb("kposTs", [128, 64]); K.b_pos = P.buf()
        K.epsc = sb("epsc", [128, 2]); K.b_eps = P.buf()

        P.memset(K.ident[:], 0.0, W=[K.b_ident])
        P.op("pool", lambda e: e.affine_select(out=K.ident[:], in_=K.ident[:], pattern=[[-1, 128]], compare_op=ALU.not_equal, fill=1.0, base=0, channel_multiplier=1), [K.b_ident], [K.b_ident])
        P.copy(K.identb[:], K.ident[:], R=[K.b_ident], W=[K.b_ident])
        P.memset(K.epsc[:, 0:1], 1e-6, W=[K.b_eps]); P.memset(K.epsc[:, 1:2], 1e-5, W=[K.b_eps])
        P.dma(K.kposT[:], I["kposT"], W=[K.b_pos])
        P.dma(K.rbbc[:], bass.AP(I["router_b"].tensor, 0, [[0, 128], [1, 64]]), W=[K.b_rb])
        P.dma(K.fnwbc[:], bass.AP(I["fnw"].tensor, 0, [[0, 128], [1, D]]), W=[K.b_gbc])

        pw = st.enter_context(contextlib.ExitStack())
        K.projw = {}
        w_v = I["w_in"].rearrange("(kc p) w -> p kc w", p=128)
        for tag, srcs in [("kv", [(C_DAK, 1024), (C_DAV, 1024), (C_CK, 128), (C_CV, 128), (C_SK, 128), (C_WK, 128), (C_SV, 128), (C_WV, 128)]),
                          ("q", [(C_DAQ, 1024), (C_NQ, 512), (C_MG, 2048), (C_NG, 24)])]:
            WT = sum(wd for _, wd in srcs)
            wt = sb("w_" + tag, [128, 8, WT], BF16, pw); bw = P.buf()
            off = 0
            for c0, wd in srcs:
                for s0 in range(0, wd, 512):
                    s1 = min(wd, s0 + 512)
                    P.dma(wt[:, :, off + s0:off + s1], w_v[:, :, c0 + s0:c0 + s1], W=[bw], eng="pool")
                off += wd
            K.projw[tag] = (wt, bw)
        phase0(K)
        if debug:
            P.dma(K.dbg_mod, K.modT[:], R=[K.b_modT])
            P.dma(K.dbg_bc[:, 0, :], K.g1bc[:], R=[K.b_gbc]); P.dma(K.dbg_bc[:, 1, :], K.g2bc[:], R=[K.b_gbc]); P.dma(K.dbg_bc[:, 2, :], K.fnwbc[:], R=[K.b_gbc])
        P.barrier(); P.flush(block)
        done = stop_after == "0"
        if not done:
            proj_phase(K, kv=True)
            P.barrier(); P.flush(block)
            proj_phase(K, kv=False)
            if debug:
                P.dma(K.dbg_gates, K.gates[:], R=[K.b_gates])
            P.barrier(); P.flush(block)
            done = stop_after == "A"
        pw.close()
        if not done:
            phase_da(K)
            P.barrier(); P.flush(block)
            done = stop_after == "B"
        if not done:
            phase_nsa(K)
            P.barrier(); P.flush(block)
            done = stop_after == "C"
        if not done:
            phase_d(K)
            if debug:
                P.dma(K.dbg_wr, K.Wr[:], R=[K.b_Wr])
            P.barrier(); P.flush(block)
            done = stop_after == "D"
        if not done:
            phase_moe(K, out_d)
        else:
            z = sb("zdbg", [128, D])
            bz = P.buf()
            P.memset(z[:], 0.0, W=[bz])
            P.dma(out_d[0:128, :], z[:], R=[bz])
        P.barrier()
        P.flush(block)
    return nc


def phase0(K):
    nc, P, I, ps, pb = K.nc, K.P, K.I, K.ps, K.pb
    with contextlib.ExitStack() as ph:
        sb = lambda n, s, d=F32: K.sb(n, s, d, ph)
        adaw = [sb(f"adaw{i}", [128, 8, 512]) for i in range(2)]; b_adaw = P.bufs(2)
        cTt = sb("cTt", [128, 8]); cact = sb("cact", [128, 8]); b_c = P.buf()
        cb = sb("cb", [128, 8, 128]); b_cb = P.buf()
        ones = sb("ones0", [128, 128]); b_ones = P.buf()
        adabT = sb("adabT", [128, 48]); n1wT = sb("n1wT_s", [128, 8]); n2wT = sb("n2wT_s", [128, 8]); b_small = P.buf()
        adabbc = sb("adabbc", [128, 2, D]); b_abc = P.buf()
        lv = sb("lv", [128, 4, 64]); lt = sb("lt", [128, 2, 64]); ls = sb("ls", [128, 2]); le = sb("le", [128, 2]); b_l = P.buf()
        swt = sb("swt", [128, 128])

        P.dma(cTt[:], I["cT"], W=[b_c])
        P.dma(adabT[:], I["ada_bT"], W=[b_small]); P.dma(n1wT[:], I["n1wT"], W=[b_small]); P.dma(n2wT[:], I["n2wT"], W=[b_small])
        P.dma(adabbc[:, 0, :], bass.AP(I["ada_b"].tensor, 2 * D, [[0, 128], [1, D]]), W=[b_abc])
        P.dma(adabbc[:, 1, :], bass.AP(I["ada_b"].tensor, 5 * D, [[0, 128], [1, D]]), W=[b_abc])
        for i, nm in enumerate(["lq1", "lk1", "lq2", "lk2"]):
            P.dma(lv[:, i, :], bass.AP(I[nm].tensor, 0, [[0, 128], [1, 64]]), W=[b_l])
        P.dma(swt[:], bass.AP(I["subln"].tensor, 0, [[0, 128], [1, 128]]), W=[K.b_sw])
        P.ts(K.swbc[:], swt[:], 0.8, None, ALU.mult, R=[K.b_sw], W=[K.b_sw])
        P.tt(lt[:, 0, :], lv[:, 0, :], lv[:, 1, :], ALU.mult, R=[b_l], W=[b_l])
        P.tt(lt[:, 1, :], lv[:, 2, :], lv[:, 3, :], ALU.mult, R=[b_l], W=[b_l])
        P.op("dve", lambda e: e.tensor_reduce(out=ls[:], in_=lt[:], axis=AX.X, op=ALU.add), [b_l], [b_l])
        P.act(le[:], ls[:], AF.Exp, R=[b_l], W=[b_l])
        P.tt(K.nlam[:], le[:, 1:2], le[:, 0:1], ALU.subtract, R=[b_l], W=[K.b_lam])
        P.ts(K.nlam[:], K.nlam[:], -0.2, None, ALU.add, R=[K.b_lam], W=[K.b_lam])
        P.act(cact[:], cTt[:], AF.Silu, R=[b_c], W=[b_c])
        P.memset(ones[:], 1.0, W=[b_ones])
        for kc in range(8):
            P.act(cb[:, kc, :], ones[:], AF.Identity, R=[b_ones, b_c], W=[b_cb], scale=cact[:, kc:kc + 1])
        adaw_v = I["ada_w"].rearrange("(kc p) w -> p kc w", p=128)
        rot = Rot(range(8))
        for blk in range(12):
            sl = blk % 2
            P.dma(adaw[sl][:], adaw_v[:, :, blk * 512:(blk + 1) * 512], W=[b_adaw[sl]])
            bank = rot.next()
            if blk in (4, 5, 10, 11):
                for kc in range(8):
                    P.mm(ps[:, bank, :], cb[:, kc, :], adaw[sl][:, kc, :], kc == 0, kc == 7, R=[b_cb, b_adaw[sl]], W=[pb[bank]])
                which = 0 if blk < 6 else 1
                half = blk % 2
                dst = (K.g1bc if which == 0 else K.g2bc)[:, half * 512:(half + 1) * 512]
                P.tt(dst, ps[:, bank, :], adabbc[:, which, half * 512:(half + 1) * 512], ALU.add, R=[pb[bank], b_abc], W=[K.b_gbc])
            else:
                for cc in range(4):
                    for kc in range(8):
                        P.mm(ps[:, bank, cc:cc + 1], adaw[sl][:, kc, cc * 128:(cc + 1) * 128], cact[:, kc:kc + 1], kc == 0, kc == 7, R=[b_c, b_adaw[sl]], W=[pb[bank]])
                P.tt(K.modT[:, blk * 4:(blk + 1) * 4], ps[:, bank, 0:4], adabT[:, blk * 4:(blk + 1) * 4], ALU.add, R=[pb[bank], b_small], W=[K.b_modT])
        P.stt(K.a1[:], K.modT[:, 8:16], 1.0, n1wT[:], ALU.add, ALU.mult, R=[K.b_modT, b_small], W=[K.b_a])
        P.stt(K.a2[:], K.modT[:, 32:40], 1.0, n2wT[:], ALU.add, ALU.mult, R=[K.b_modT, b_small], W=[K.b_a])
        P.barrier()
        P.flush(K.block)


def norm_chunk(K, xt, b_xt, hT, b_hT, ss, sd, rs, b_st, junk, b_junk, a_ap, sh_ap, rot, hT_lo=None):
    P, ps, pb = K.P, K.ps, K.pb
    for sub in range(4):
        P.act(junk[:], xt[:, sub, :], AF.Square, R=[b_xt], W=[b_junk, b_st], accum_out=ss[:, sub:sub + 1])
    P.act(sd[:], ss[:], AF.Sqrt, R=[b_st, K.b_eps], W=[b_st], scale=1.0 / D, bias=K.epsc[:, 0:1])
    P.op("dve", lambda e: e.reciprocal(out=rs[:], in_=sd[:]), [b_st], [b_st])
    for sub in range(4):
        P.ts(xt[:, sub, :], xt[:, sub, :], rs[:, sub:sub + 1], None, ALU.mult, R=[b_st, b_xt], W=[b_xt])
    for fc in range(8):
        bank = rot.next()
        for sub in range(4):
            P.tr(ps[:, bank, sub * 128:(sub + 1) * 128], xt[:, sub, fc * 128:(fc + 1) * 128], K.ident[:], R=[b_xt, K.b_ident], W=[pb[bank]])
        if hT_lo is None:
            P.act(hT[:, fc, :], ps[:, bank, :], AF.Identity, R=[pb[bank], K.b_a, K.b_modT], W=[b_hT], scale=a_ap[:, fc:fc + 1], bias=sh_ap[:, fc:fc + 1])
        else:
            hfl, b_hfl, lo, b_lo = hT_lo
            hf = hfl[fc % 2]; b_hf = b_hfl[fc % 2]
            P.act(hf[:], ps[:, bank, :], AF.Identity, R=[pb[bank], K.b_a, K.b_modT], W=[b_hf], scale=a_ap[:, fc:fc + 1], bias=sh_ap[:, fc:fc + 1])
            P.copy(hT[:, fc, :], hf[:], R=[b_hf], W=[b_hT])
            P.tt(lo[:, fc, :], hf[:], hT[:, fc, :], ALU.subtract, R=[b_hf, b_hT], W=[b_lo])


def proj_phase(K, kv):
    nc, P, I, Sc, ps, pb = K.nc, K.P, K.I, K.Sc, K.ps, K.pb
    with contextlib.ExitStack() as ph:
        sb = lambda n, s, d=F32: K.sb(n, s, d, ph)
        tag = "kv" if kv else "q"
        if kv:
            srcs = [(C_DAK, 1024), (C_DAV, 1024), (C_CK, 128), (C_CV, 128), (C_SK, 128), (C_WK, 128), (C_SV, 128), (C_WV, 128)]
            x_d, nch = I["xn"], 16
        else:
            srcs = [(C_DAQ, 1024), (C_NQ, 512), (C_MG, 2048), (C_NG, 24)]
            x_d, nch = I["xo"], 8
        w, b_w = K.projw[tag]
        xt = [sb(f"xt{i}_" + tag, [128, 4, D]) for i in range(2)]; b_xt = P.bufs(2)
        hT = [sb(f"hT{i}_" + tag, [128, 8, 512], BF16) for i in range(2)]; b_hT = P.bufs(2)
        ss = [sb(f"ss{i}_" + tag, [128, 4]) for i in range(2)]; sd = [sb(f"sd{i}_" + tag, [128, 4]) for i in range(2)]
        rs = [sb(f"rs{i}_" + tag, [128, 4]) for i in range(2)]; b_st = P.bufs(2)
        junk = sb("junk_" + tag, [128, D], BF16); b_junk = P.buf()
        NST = 6
        stg = [sb(f"stg{i}_" + tag, [128, 512], BF16) for i in range(NST)]; b_stg = P.bufs(NST)
        srot = Rot(range(NST))
        if not kv:
            stgf = [sb(f"stgf{i}", [128, 512]) for i in range(4)]; b_stgf = P.bufs(4)
            frot = Rot(range(4))
        rot = Rot(range(8))
        x_v = x_d.rearrange("(c s r) d -> c r s d", s=4, r=128)

        def load(c):
            P.dma(xt[c % 2][:], x_v[c], W=[b_xt[c % 2]])
        load(0)
        evq = Rot(["dve", "act"])
        for c in range(nch):
            if c + 1 < nch:
                load(c + 1)
            s = c % 2
            norm_chunk(K, xt[s], b_xt[s], hT[s], b_hT[s], ss[s], sd[s], rs[s], b_st[s], junk, b_junk, K.a1, K.modT[:, 0:8], rot)
            h = hT[s]
            tok0 = c * 512

            def fm_group(col, dst_ap, mode):
                bank = rot.next()
                for kc in range(8):
                    P.mm(ps[:, bank, :], w[:, kc, col:col + 128], h[:, kc, :], kc == 0, kc == 7, R=[b_w, b_hT[s]], W=[pb[bank]])
                if mode == "sig":
                    j = frot.next()
                    P.act(stgf[j][:], ps[:, bank, :], AF.Sigmoid, R=[pb[bank]], W=[b_stgf[j]])
                    P.dma(dst_ap, stgf[j][:], R=[b_stgf[j]])
                else:
                    j = srot.next()
                    eng = evq.next()
                    if mode == "q":
                        if eng == "act":
                            P.act(stg[j][:], ps[:, bank, :], AF.Copy, R=[pb[bank]], W=[b_stg[j]], scale=0.125)
                        else:
                            P.ts(stg[j][:], ps[:, bank, :], 0.125, None, ALU.mult, R=[pb[bank]], W=[b_stg[j]])
                    else:
                        if eng == "act":
                            P.act(stg[j][:], ps[:, bank, :], AF.Copy, R=[pb[bank]], W=[b_stg[j]])
                        else:
                            P.copy(stg[j][:], ps[:, bank, :], R=[pb[bank]], W=[b_stg[j]])
                    P.dma(dst_ap, stg[j][:], R=[b_stg[j]])

            if kv:
                for hh in range(8):
                    fm_group(hh * 128, Sc["ktda"][hh, :, tok0:tok0 + 512], "k")
                for gi in range(4):
                    fm_group(2048 + gi * 128, Sc["nfm"][gi, :, tok0:tok0 + 512], "k")
                for sub in range(4):
                    for (col, wd, dst) in [(1024, 512, Sc["vda"][tok0 + sub * 128:tok0 + (sub + 1) * 128, 0:512]),
                                           (1536, 512, Sc["vda"][tok0 + sub * 128:tok0 + (sub + 1) * 128, 512:1024]),
                                           (2560, 256, Sc["ntm"][tok0 + sub * 128:tok0 + (sub + 1) * 128, :])]:
                        bank = rot.next()
                        for kc in range(8):
                            P.mm(ps[:, bank, 0:wd], h[:, kc, sub * 128:(sub + 1) * 128], w[:, kc, col:col + wd], kc == 0, kc == 7, R=[b_w, b_hT[s]], W=[pb[bank]])
                        j = srot.next()
                        eng = evq.next()
                        if eng == "act":
                            P.act(stg[j][:, 0:wd], ps[:, bank, 0:wd], AF.Copy, R=[pb[bank]], W=[b_stg[j]])
                        else:
                            P.copy(stg[j][:, 0:wd], ps[:, bank, 0:wd], R=[pb[bank]], W=[b_stg[j]])
                        P.dma(dst, stg[j][:, 0:wd], R=[b_stg[j]])
            else:
                for hh in range(8):
                    fm_group(hh * 128, Sc["qtda"][hh, :, tok0:tok0 + 512], "q")
                for pr in range(4):
                    fm_group(1024 + pr * 128, Sc["qtn"][pr, :, tok0:tok0 + 512], "q")
                for fc in range(16):
                    fm_group(1536 + fc * 128, Sc["mg"][fc, :, tok0:tok0 + 512], "sig")
                for sub in range(4):
                    bank = rot.next()
                    for kc in range(8):
                        P.mm(ps[:, bank, 0:24], h[:, kc, sub * 128:(sub + 1) * 128], w[:, kc, 3584:3608], kc == 0, kc == 7, R=[b_w, b_hT[s]], W=[pb[bank]])
                    P.act(K.gates[:, c * 4 + sub, :], ps[:, bank, 0:24], AF.Sigmoid, R=[pb[bank]], W=[K.b_gates])
        P.barrier()
        P.flush(K.block)


class Job:
    __slots__ = ("kt", "qt", "masks", "bias", "v", "W", "acc", "first", "last", "RS", "RB", "RV", "epi", "pre", "subs", "fs", "ls")


def run_attn(K, jobs, PT, b_PT, sbanks):
    P, ps, pb = K.P, K.ps, K.pb
    n = len(jobs)
    LA = len(sbanks) - 1
    srot = Rot(sbanks)
    prot = Rot(range(len(PT)))
    slot_of = {}

    def issue_S(i):
        jb = jobs[i]
        if jb.pre is not None:
            jb.pre()
        sbk = srot.next()
        c0 = 128 * min(jb.subs)
        c1 = 128 * (max(jb.subs) + 1)
        P.mm(ps[:, sbk, c0:c1], jb.kt, jb.qt[:, c0:c1], True, len(jb.masks) == 0, R=jb.RS, W=[pb[sbk]])
        for mi, (ml, mr, mR) in enumerate(jb.masks):
            P.mm(ps[:, sbk, c0:c1], ml, mr[:, c0:c1], False, mi == len(jb.masks) - 1, R=mR, W=[pb[sbk]])
        sl = prot.next()
        slot_of[i] = sl
        P.act(PT[sl][:, c0:c1], ps[:, sbk, c0:c1], AF.Exp, R=[pb[sbk]] + jb.RB, W=[b_PT[sl]], bias=jb.bias, scale=1.0)

    def issue_PV(i):
        jb = jobs[i]
        sl = slot_of.pop(i)
        for sub in jb.subs:
            f = jb.first if jb.fs is None else jb.fs[sub]
            l = jb.last if jb.ls is None else jb.ls[sub]
            P.mm(ps[:, jb.acc[sub], 0:jb.W], PT[sl][:, sub * 128:(sub + 1) * 128], jb.v, f, l, R=[b_PT[sl]] + jb.RV, W=[pb[jb.acc[sub]]])
        if jb.epi is not None:
            jb.epi()

    for step in range(n + LA):
        if step < n:
            issue_S(step)
        if step >= LA:
            issue_PV(step - LA)


def band_subs(kt, m, window=False):
    out = []
    for i in range(4):
        if 2 * i + 1 < kt:
            continue
        if m * max(0, 128 * (2 * i - kt) - 127) > SKIP_THR:
            continue
        if window and 2 * i - kt >= 5:
            continue
        out.append(i)
    return out


def assign_flags(group):
    first = {}
    last = {}
    for idx, jb in enumerate(group):
        for sub in jb.subs:
            first.setdefault(sub, idx)
            last[sub] = idx
    assert sorted(first) == [0, 1, 2, 3]
    for idx, jb in enumerate(group):
        jb.fs = {sub: first[sub] == idx for sub in jb.subs}
        jb.ls = {sub: last[sub] == idx for sub in jb.subs}


def mkjob(kt, qt, masks, bias, v, W, acc, first, last, RS, RB, RV, epi=None):
    j = Job()
    j.kt, j.qt, j.masks, j.bias, j.v, j.W, j.acc = kt, qt, masks, bias, v, W, acc
    j.first, j.last, j.RS, j.RB, j.RV, j.epi = first, last, RS, RB, RV, epi
    j.pre = None
    j.subs = [0, 1, 2, 3]
    j.fs = None
    j.ls = None
    return j


def phase_da(K):
    nc, P, I, Sc, ps, pb = K.nc, K.P, K.I, K.Sc, K.ps, K.pb
    with contextlib.ExitStack() as ph:
        sb = lambda n, s, d=F32: K.sb(n, s, d, ph)
        KT = [[sb(f"KT{c}{i}", [128, S], BF16) for i in range(2)] for c in range(2)]
        b_KT = [[P.buf() for i in range(2)] for c in range(2)]
        QT = [[sb(f"QT{c}{i}", [128, NQ], BF16) for i in range(2)] for c in range(2)]
        b_QT = [[P.buf() for i in range(2)] for c in range(2)]
        V = [sb(f"Vd{i}", [128, 64, 129], BF16) for i in range(2)]; b_V = P.bufs(2)
        bK = [sb(f"bK{i}", [128, 64]) for i in range(2)]; b_bK = P.bufs(2)
        dm = sb("dmask_s", [128, 8, 512], BF16); b_dm = P.buf()
        PT = [sb(f"PT{i}", [128, 512], BF16) for i in range(6)]; b_PT = P.bufs(6)
        o1 = sb("o1", [128, 4, 128]); b_o1 = P.buf()
        oo = [sb(f"oo{i}", [128, 4, 128]) for i in range(2)]; b_oo = P.bufs(2)
        on = [sb(f"on{i}", [128, 4, 128]) for i in range(2)]; b_on = P.bufs(2)
        rz = [sb(f"rz{i}", [128, 8]) for i in range(2)]; b_rz = P.bufs(2)
        ssq = [sb(f"ssq{i}", [128, 12]) for i in range(2)]; b_ssq = P.bufs(2)
        jk = sb("jk", [128, 128]); b_jk = P.buf()
        nh = sb("nh", [128, 4]); b_nh = P.buf()
        ost = [sb(f"ost{i}", [128, 512], BF16) for i in range(2)]; b_ost = P.bufs(2)
        P.memset(nh[:], -0.5, W=[b_nh])
        P.dma(dm[:], I["dmask"], W=[b_dm])
        for c in range(2):
            for i in range(2):
                P.memset(KT[c][i][64:65, :], 1.0, W=[b_KT[c][i]])
        for i in range(2):
            P.memset(V[i][:, :, 128:129], 1.0, W=[b_V[i]], eng="dve")
        vda_v = Sc["vda"].rearrange("(n r) d -> r n d", r=128)

        def load(hh):
            s = hh % 2
            m = float(SL_A[hh])
            for c in range(2):
                P.dma(KT[c][s][0:64, :], Sc["ktda"][hh, c * 64:(c + 1) * 64, :], W=[b_KT[c][s]])
                P.dma(QT[c][s][0:64, :], Sc["qtda"][hh, c * 64:(c + 1) * 64, :], W=[b_QT[c][s]])
                P.dma(QT[c][s][64:65, :], I["refda"][hh:hh + 1, :], W=[b_QT[c][s]])
            P.dma(V[s][:, :, 0:128], vda_v[:, :, hh * 128:(hh + 1) * 128], W=[b_V[s]])
            P.ts(bK[s][:], K.kposT[:], m, None, ALU.mult, R=[K.b_pos], W=[b_bK[s]], eng="pool")

        accb = [3, 4, 5, 6]
        TB = 7
        state = {"k": 0, "deferred": None, "e": 0, "short": False}
        ev = [sb(f"evd{i}", [128, 4, 129]) for i in range(3)]; b_ev = P.bufs(3)

        def make_epi(hh, j, comp, short):
            s = hh % 2

            def epi():
                state["short"] = short
                k = state["k"] % 2
                x = state["e"] % 3
                state["e"] += 1
                for sub in range(4):
                    if state["short"]:
                        P.act(ev[x][:, sub, :], ps[:, accb[sub], 0:129], AF.Copy, R=[pb[accb[sub]]], W=[b_ev[x]])
                    else:
                        P.copy(ev[x][:, sub, :], ps[:, accb[sub], 0:129], R=[pb[accb[sub]]], W=[b_ev[x]])
                if comp == 0:
                    for sub in range(4):
                        P.op("dve", lambda e, sub=sub: e.reciprocal(out=rz[k][:, sub:sub + 1], in_=ev[x][:, sub, 128:129]), [b_ev[x]], [b_rz[k]])
                        P.ts(o1[:, sub, :], ev[x][:, sub, 0:128], rz[k][:, sub:sub + 1], None, ALU.mult, R=[b_ev[x], b_rz[k]], W=[b_o1])
                    return
                for sub in range(4):
                    P.op("dve", lambda e, sub=sub: e.reciprocal(out=rz[k][:, 4 + sub:5 + sub], in_=ev[x][:, sub, 128:129]), [b_ev[x]], [b_rz[k]])
                    P.tt(rz[k][:, 4 + sub:5 + sub], rz[k][:, 4 + sub:5 + sub], K.nlam[:], ALU.mult, R=[b_rz[k], K.b_lam], W=[b_rz[k]])
                    P.stt(oo[k][:, sub, :], ev[x][:, sub, 0:128], rz[k][:, 4 + sub:5 + sub], o1[:, sub, :], ALU.mult, ALU.add, R=[b_ev[x], b_rz[k], b_o1], W=[b_oo[k]])
                for sub in range(4):
                    P.op("dve", lambda e, sub=sub: e.scalar_tensor_tensor(out=jk[:], in0=oo[k][:, sub, :], scalar=1.0, in1=oo[k][:, sub, :], op0=ALU.mult, op1=ALU.mult, accum_out=ssq[k][:, sub:sub + 1]), [b_oo[k]], [b_jk, b_ssq[k]])
                P.ts(ssq[k][:, 4:8], ssq[k][:, 0:4], 1.0 / 128.0, 1e-5, ALU.mult, ALU.add, R=[b_ssq[k]], W=[b_ssq[k]])
                P.tt(ssq[k][:, 8:12], ssq[k][:, 4:8], nh[:], ALU.pow, R=[b_ssq[k], b_nh], W=[b_ssq[k]], eng="pool")
                for sub in range(4):
                    P.stt(on[k][:, sub, :], oo[k][:, sub, :], ssq[k][:, 8 + sub:9 + sub], K.swbc[:], ALU.mult, ALU.mult, R=[b_oo[k], b_ssq[k], K.b_sw], W=[b_on[k]])

                def deferred(k=k, hh=hh, j=j):
                    for sub in range(4):
                        P.tr(ps[:, TB, sub * 128:(sub + 1) * 128], on[k][:, sub, :], K.ident[:], R=[b_on[k], K.b_ident], W=[pb[TB]])
                    P.copy(ost[k][:], ps[:, TB, :], R=[pb[TB]], W=[b_ost[k]])
                    P.dma(Sc["oat"][hh, :, j * 512:(j + 1) * 512], ost[k][:], R=[b_ost[k]])
                prev = state["deferred"]
                state["deferred"] = deferred
                state["k"] += 1
                if prev is not None:
                    prev()
            return epi

        load(0)
        for hh in range(8):
            if hh + 1 < 8:
                load(hh + 1)
            s = hh % 2
            jobs = []
            for j in range(8):
                ntile = 8 * j + 8
                tf = first_tile(j, float(SL_A[hh]))
                for comp in range(2):
                    grp = []
                    for t in range(0, ntile):
                        subs = band_subs(t - 8 * j, float(SL_A[hh]))
                        if not subs:
                            continue
                        masks = []
                        if t >= 8 * j:
                            masks = [(K.identb[:], dm[:, t - 8 * j, :], [K.b_ident, b_dm])]
                        jb = mkjob(KT[comp][s][0:65, t * 128:(t + 1) * 128], QT[comp][s][0:65, j * 512:(j + 1) * 512], masks,
                                   bK[s][:, t:t + 1], V[s][:, t, :], 129, accb, t == tf, t == ntile - 1,
                                   [b_KT[comp][s], b_QT[comp][s]], [b_bK[s]], [b_V[s]],
                                   None)
                        jb.subs = subs
                        grp.append(jb)
                    assign_flags(grp)
                    grp[-1].epi = make_epi(hh, j, comp, len(grp) < 20)
                    jobs.extend(grp)
            run_attn(K, jobs, PT, b_PT, [0, 1, 2])
        if state["deferred"] is not None:
            state["deferred"]()
        P.barrier()
        P.flush(K.block)


def phase_nsa(K):
    nc, P, I, Sc, ps, pb = K.nc, K.P, K.I, K.Sc, K.ps, K.pb
    with contextlib.ExitStack() as ph:
        sb = lambda n, s, d=F32, stk=ph: K.sb(n, s, d, stk)
        KS = sb("KS", [128, S], BF16); KW = sb("KW", [128, S], BF16); b_KS = P.buf(); b_KW = P.buf()
        VS = sb("VS", [128, 64, 65], BF16); VW = sb("VW", [128, 64, 65], BF16); b_VS = P.buf(); b_VW = P.buf()
        Qh = [sb(f"Qh{i}", [128, NQ], BF16) for i in range(4)]; b_Qh = P.bufs(4)
        KC = sb("KC", [128, 512], BF16); b_KC = P.buf()
        VC = sb("VC", [128, 4, 193], BF16); b_VC = P.buf()
        dm = sb("dm_n", [128, 8, 512], BF16); wm = sb("wm_n", [128, 12, 512], BF16)
        QW = [[sb(f"QW{w_}{h_}", [128, 512], BF16) for h_ in range(4)] for w_ in range(4)]
        b_QW = [[P.buf() for h_ in range(4)] for w_ in range(4)]
        cm = sb("cm_n", [128, 3, 512], BF16); addm = sb("addm", [128, 32, 128], BF16); b_const = P.buf()
        cposT = sb("cposT_s", [128, 4])
        PT = [sb(f"PTn{i}", [128, 512], BF16) for i in range(6)]; b_PT = P.bufs(6)
        bKs = sb("bKs", [128, 4, 64]); bC = sb("bC", [128, 4, 4]); b_bias = P.buf()
        ocomb = [sb(f"ocomb{i}", [128, 4, 4, 64]) for i in range(2)]; b_oc = P.bufs(2)
        imp = sb("imp", [128, 4, 128]); b_imp = P.buf()
        scr = sb("scr", [128, 128]); scr2 = sb("scr2", [128, 128]); t8 = sb("t8", [128, 16]); b_sel = P.buf()
        selb = sb("selb", [128, 4, 128]); b_selb = P.buf()
        rzt = sb("rzt", [128, 8]); b_rzt = P.buf()
        evn = [sb(f"evn{i}", [128, 4, 193]) for i in range(3)]; b_evn = P.bufs(3)
        est = {"e": 0}
        obst = [sb(f"obst{i}", [128, 512], BF16) for i in range(2)]; b_obst = P.bufs(2)
        P.dma(dm[:], I["dmask"], W=[b_const]); P.dma(wm[:], I["wmask"], W=[b_const])
        P.dma(cm[:], I["cmask"], W=[b_const]); P.dma(addm[:], I["addmask"], W=[b_const]); P.dma(cposT[:], I["cposT"], W=[b_const])
        P.memset(KS[64:96, :], 0.0, W=[b_KS]); P.memset(KS[64:65, :], 1.0, W=[b_KS]); P.memset(KW[64:65, :], 1.0, W=[b_KW])
        P.dma(KS[96:128, :], I["wind"], W=[b_KS])
        for w_ in range(4):
            for h_ in range(4):
                P.memset(QW[w_][h_][64:96, :], 0.0, W=[b_QW[w_][h_]])
        P.memset(VS[:, :, 64:65], 1.0, W=[b_VS], eng="dve"); P.memset(VW[:, :, 64:65], 1.0, W=[b_VW], eng="dve")
        P.memset(VC[:, :, 192:193], 1.0, W=[b_VC], eng="dve")
        P.dma(VC[:, :, 64:192], I["c2s"], W=[b_VC])
        ntm_v = Sc["ntm"].rearrange("(n r) d -> r n d", r=128)
        accb = [3, 4, 5, 6]
        TB = 7
        rot = Rot([3, 4, 5, 6, 7])

        ctmp = {}
        cbufs = P.bufs(5)
        for g in range(2):
            P.dma(VS[:, :, 0:64], ntm_v[:, :, g * 64:(g + 1) * 64], W=[b_VS])
            P.dma(VW[:, :, 0:64], ntm_v[:, :, 128 + g * 64:128 + (g + 1) * 64], W=[b_VW])
            for hg in range(4):
                hh = g * 4 + hg
                m = float(SL_B[hh])
                P.dma(Qh[hg][0:64, :], Sc["qtn"][hh // 2, (hh % 2) * 64:(hh % 2 + 1) * 64, :], W=[b_Qh[hg]])
                P.dma(Qh[hg][64:65, :], I["refn"][hh:hh + 1, :], W=[b_Qh[hg]])
                P.ts(bKs[:, hg, :], K.kposT[:], m, None, ALU.mult, R=[K.b_pos], W=[b_bias], eng="pool")
                P.ts(bC[:, hg, :], cposT[:], m, None, ALU.mult, R=[b_const], W=[b_bias], eng="pool")
            P.memset(KC[:], 0.0, W=[b_KC]); P.memset(KC[64:65, :], 1.0, W=[b_KC])
            for kind in range(2):
                if True:
                    def sbc(n, s, d=F32):
                        if n not in ctmp:
                            ctmp[n] = K.sb("c_" + n, s, d, ph)
                        return ctmp[n]
                    cT = KS if kind == 0 else KW; w1 = sbc("w1", [128, 32, 128], BF16); w2 = sbc("w2", [128, 64], BF16)
                    peT = sbc("peT", [128, 32], BF16); hf = sbc("hf", [128, 512]); t1 = sbc("t1", [128, 512])
                    hid = sbc("hid", [128, 512], BF16); cst = sbc("cst", [128, 1])
                    b_c = b_KS if kind == 0 else b_KW
                    b_w, b_h, b_t, b_hid, b_cst = cbufs
                    pre = "ck" if kind == 0 else "cv"
                    P.dma(cT[0:64, :], Sc["nfm"][kind, g * 64:(g + 1) * 64, :], W=[b_c])
                    P.dma(w1[0:64, :, :], I[pre + "_w1"].rearrange("(l d) h -> d l h", d=64), W=[b_w], eng="pool")
                    P.dma(w2[:], I[pre + "_w2"], W=[b_w], eng="pool")
                    P.dma(peT[0:64, :], I[pre + "_peT"], W=[b_w], eng="pool")
                    bank = rot.next()
                    for l in range(32):
                        P.mm(ps[:, bank, 0:1], w1[0:64, l, :], peT[0:64, l:l + 1], l == 0, l == 31, R=[b_w], W=[pb[bank]])
                    P.copy(cst[:], ps[:, bank, 0:1], R=[pb[bank]], W=[b_cst])
                    bank = rot.next()
                    cTr = cT[0:64, :].rearrange("p (c s) -> p c s", s=16)
                    for l in range(32):
                        rhs = cTr[:, 0:511, l] if l < 16 else cTr[:, 1:512, l - 16]
                        P.mm(ps[:, bank, 0:511], w1[0:64, l, :], rhs, l == 0, l == 31, R=[b_w, b_c], W=[pb[bank]])
                    P.memset(hf[:], 0.0, W=[b_h])
                    P.act(hf[:, 0:511], ps[:, bank, 0:511], AF.Identity, R=[pb[bank], b_cst], W=[b_h], bias=cst[:, 0:1], scale=1.0)
                    P.tt(t1[:], hf[:], hf[:], ALU.mult, R=[b_h], W=[b_t])
                    P.ts(t1[:], t1[:], 0.044715, 1.0, ALU.mult, ALU.add, R=[b_t], W=[b_t])
                    P.tt(t1[:], t1[:], hf[:], ALU.mult, R=[b_t, b_h], W=[b_t])
                    P.act(t1[:], t1[:], AF.Sigmoid, R=[b_t], W=[b_t], scale=1.5957691216057308)
                    P.tt(hid[:], hf[:], t1[:], ALU.mult, R=[b_t, b_h], W=[b_hid])
                    if kind == 0:
                        bank = rot.next()
                        P.mm(ps[0:64, bank, 0:511], w2[:, 0:64], hid[:, 0:511], True, True, R=[b_w, b_hid], W=[pb[bank]])
                        P.copy(KC[0:64, 0:511], ps[0:64, bank, 0:511], R=[pb[bank]], W=[b_KC])
                    else:
                        bank = rot.next()
                        for tau in range(4):
                            P.mm(ps[:, bank, tau * 64:(tau + 1) * 64], hid[:, tau * 128:(tau + 1) * 128], w2[:, :], True, True, R=[b_w, b_hid], W=[pb[bank]])
                        P.copy(VC[:, :, 0:64], ps[:, bank, 0:256].rearrange("p (t d) -> p t d", d=64), R=[pb[bank]], W=[b_VC])
            P.dma(KS[0:64, :], Sc["nfm"][2, g * 64:(g + 1) * 64, :], W=[b_KS])
            P.dma(KW[0:64, :], Sc["nfm"][3, g * 64:(g + 1) * 64, :], W=[b_KW])
            if K.debug:
                P.dma(K.dbg_kc[g], KC[0:64, :], R=[b_KC]); P.dma(K.dbg_vc[g], VC[:, :, 0:64], R=[b_VC])
            state = {"deferred": None}
            jobs = []
            for j in range(8):
                k = j % 2
                qs = slice(j * 512, (j + 1) * 512)

                def cmp_epi(hg, j=j, k=k):
                    def epi():
                        hh = g * 4 + hg
                        x = est["e"] % 3
                        est["e"] += 1
                        for sub in range(4):
                            P.act(evn[x][:, sub, :], ps[:, accb[sub], 0:193], AF.Copy, R=[pb[accb[sub]]], W=[b_evn[x]])
                        for sub in range(4):
                            tile = j * 4 + sub
                            P.ts(rzt[:, sub:sub + 1], evn[x][:, sub, 192:193], 1e-30, None, ALU.max, R=[b_evn[x]], W=[b_rzt])
                            P.op("dve", lambda e, sub=sub: e.reciprocal(out=rzt[:, sub:sub + 1], in_=rzt[:, sub:sub + 1]), [b_rzt], [b_rzt])
                            if hg == 0:
                                P.ts(imp[:, sub, :], evn[x][:, sub, 64:192], rzt[:, sub:sub + 1], None, ALU.mult, R=[b_evn[x], b_rzt], W=[b_imp])
                            else:
                                P.stt(imp[:, sub, :], evn[x][:, sub, 64:192], rzt[:, sub:sub + 1], imp[:, sub, :], ALU.mult, ALU.add, R=[b_evn[x], b_rzt, b_imp], W=[b_imp])
                            P.ts(ocomb[k][:, sub, hg, :], evn[x][:, sub, 0:64], rzt[:, sub:sub + 1], K.gates[:, tile, hh * 3:hh * 3 + 1], ALU.mult, ALU.mult, R=[b_evn[x], b_rzt, K.b_gates], W=[b_oc[k]])
                        if hg == 3:
                            for sub in range(4):
                                tile = j * 4 + sub
                                P.tt(scr[:], imp[:, sub, :], addm[:, tile, :], ALU.add, R=[b_imp, b_const], W=[b_sel])
                                P.op("dve", lambda e: e.max(out=t8[:, 0:8], in_=scr[:]), [b_sel], [b_sel])
                                P.op("dve", lambda e: e.match_replace(out=scr2[:], in_to_replace=t8[:, 0:8], in_values=scr[:], imm_value=-3.0e38), [b_sel], [b_sel])
                                P.op("dve", lambda e: e.max(out=t8[:, 8:16], in_=scr2[:]), [b_sel], [b_sel])
                                P.ts(selb[:, sub, :], scr[:], t8[:, 15:16], NEG, ALU.is_lt, ALU.mult, R=[b_sel], W=[b_selb])
                    return epi

                def br_epi(hg, br, short, j=j, k=k):
                    def epi():
                        hh = g * 4 + hg
                        x = est["e"] % 3
                        est["e"] += 1
                        for sub in range(4):
                            if short:
                                P.act(evn[x][:, sub, 0:65], ps[:, accb[sub], 0:65], AF.Copy, R=[pb[accb[sub]]], W=[b_evn[x]])
                            else:
                                P.copy(evn[x][:, sub, 0:65], ps[:, accb[sub], 0:65], R=[pb[accb[sub]]], W=[b_evn[x]])
                        for sub in range(4):
                            tile = j * 4 + sub
                            c = 4 + sub
                            P.op("dve", lambda e, sub=sub, c=c: e.reciprocal(out=rzt[:, c:c + 1], in_=evn[x][:, sub, 64:65]), [b_evn[x]], [b_rzt])
                            P.tt(rzt[:, c:c + 1], rzt[:, c:c + 1], K.gates[:, tile, hh * 3 + br:hh * 3 + br + 1], ALU.mult, R=[b_rzt, K.b_gates], W=[b_rzt])
                            P.stt(ocomb[k][:, sub, hg, :], evn[x][:, sub, 0:64], rzt[:, c:c + 1], ocomb[k][:, sub, hg, :], ALU.mult, ALU.add, R=[b_evn[x], b_rzt, b_oc[k]], W=[b_oc[k]])
                        if br == 1 and hg == 3:
                            def deferred(j=j, k=k):
                                for pp in range(2):
                                    for sub in range(4):
                                        P.tr(ps[:, TB, sub * 128:(sub + 1) * 128], ocomb[k][:, sub, 2 * pp:2 * pp + 2, :].rearrange("p a b -> p (a b)"), K.ident[:], R=[b_oc[k], K.b_ident], W=[pb[TB]])
                                    o = (j * 2 + pp) % 2
                                    P.copy(obst[o][:], ps[:, TB, :], R=[pb[TB]], W=[b_obst[o]])
                                    P.dma(Sc["obt"][g * 2 + pp, :, j * 512:(j + 1) * 512], obst[o][:], R=[b_obst[o]])
                            prev = state["deferred"]
                            state["deferred"] = deferred
                            if prev is not None:
                                prev()
                    return epi

                def sel_pre(j=j, k=k):
                    def pre():
                        for sub in range(4):
                            P.tr(ps[:, TB, sub * 128:(sub + 1) * 128], selb[:, sub, :], K.ident[:], R=[b_selb, K.b_ident], W=[pb[TB]])
                        for hg in range(4):
                            tf = first_tile(j, float(SL_B[g * 4 + hg]))
                            for w_ in range(tf // 16, (8 * j + 7) // 16 + 1):
                                P.copy(QW[w_][hg][0:65, :], Qh[hg][0:65, j * 512:(j + 1) * 512], R=[b_Qh[hg]], W=[b_QW[w_][hg]], eng="pool")
                                P.copy(QW[w_][hg][96:128, :], ps[32 * w_:32 * w_ + 32, TB, :], R=[pb[TB]], W=[b_QW[w_][hg]])
                    return pre

                tmax = (64 * j + 62) // 128
                for hg in range(4):
                    for tau in range(tmax + 1):
                        masks = []
                        if j % 2 == 0:
                            if tau == j // 2:
                                masks = [(K.identb[:], cm[:, 1, :], [K.b_ident, b_const])]
                            elif tau == j // 2 - 1:
                                masks = [(K.identb[:], cm[:, 0, :], [K.b_ident, b_const])]
                        else:
                            if tau == (j - 1) // 2:
                                masks = [(K.identb[:], cm[:, 2, :], [K.b_ident, b_const])]
                        jobs.append(mkjob(KC[0:65, tau * 128:(tau + 1) * 128], Qh[hg][0:65, qs], masks, bC[:, hg, tau:tau + 1], VC[:, tau, :], 193, accb,
                                          tau == 0, tau == tmax, [b_KC, b_Qh[hg]], [b_bias], [b_VC], cmp_epi(hg) if tau == tmax else None))
                for hg in range(4):
                    t0 = max(0, 8 * j - 4, first_tile(j, float(SL_B[g * 4 + hg])))
                    grp = []
                    for t in range(max(0, 8 * j - 4), 8 * j + 8):
                        subs = band_subs(t - 8 * j, float(SL_B[g * 4 + hg]), window=True)
                        if not subs:
                            continue
                        masks = [(K.identb[:], wm[:, t - (8 * j - 4), :], [K.b_ident, b_const])]
                        jb = mkjob(KW[0:65, t * 128:(t + 1) * 128], Qh[hg][0:65, qs], masks, bKs[:, hg, t:t + 1], VW[:, t, :], 65, accb,
                                   t == t0, t == 8 * j + 7, [b_KW, b_Qh[hg]], [b_bias], [b_VW], None)
                        jb.subs = subs
                        grp.append(jb)
                    assign_flags(grp)
                    grp[-1].epi = br_epi(hg, 2, len(grp) < 20)
                    jobs.extend(grp)
                for hg in range(4):
                    tf = first_tile(j, float(SL_B[g * 4 + hg]))
                    sgrp = []
                    for t in range(0, 8 * j + 8):
                        subs = band_subs(t - 8 * j, float(SL_B[g * 4 + hg]))
                        if not subs:
                            continue
                        masks = []
                        if t >= 8 * j:
                            masks.append((K.identb[:], dm[:, t - 8 * j, :], [K.b_ident, b_const]))
                        jb = mkjob(KS[:, t * 128:(t + 1) * 128], QW[t // 16][hg][:, :], masks, bKs[:, hg, t:t + 1], VS[:, t, :], 65, accb,
                                   t == tf, t == 8 * j + 7, [b_KS, b_QW[t // 16][hg]], [b_bias], [b_VS], None)
                        if hg == 0 and not sgrp:
                            jb.pre = sel_pre()
                        jb.subs = subs
                        sgrp.append(jb)
                    assign_flags(sgrp)
                    sgrp[-1].epi = br_epi(hg, 1, len(sgrp) < 20)
                    jobs.extend(sgrp)
            run_attn(K, jobs, PT, b_PT, [0, 1, 2])
            if state["deferred"] is not None:
                state["deferred"]()
            P.barrier()
            P.flush(K.block)


def phase_d(K):
    nc, P, I, Sc, ps, pb = K.nc, K.P, K.I, K.Sc, K.ps, K.pb
    with contextlib.ExitStack() as ph:
        sb = lambda n, s, d=F32: K.sb(n, s, d, ph)
        wda = sb("wda", [128, 8, D], BF16); wnsa = sb("wnsa", [128, 4, D], BF16); wo = sb("wo", [128, 8, D], BF16); b_w = P.buf()
        rw = sb("rw", [128, 8, 64]); rwh = sb("rwh", [128, 8, 64], BF16); rwl = sb("rwl", [128, 8, 64], BF16); b_rw = P.buf()
        for (dst, src, n) in [(wda, I["w_da_out"], 8), (wnsa, I["w_nsa_out"], 4), (wo, I["w_o"], 8)]:
            v = src.rearrange("(h p) f -> p h f", p=128)
            for half in range(2):
                P.dma(dst[:, :, half * 512:(half + 1) * 512], v[:, :, half * 512:(half + 1) * 512], W=[b_w], eng="pool")
        for kc in range(8):
            P.tt(wo[:, kc, :], wo[:, kc, :], K.g1bc[:], ALU.mult, R=[K.b_gbc, b_w], W=[b_w])
        P.dma(rw[:], I["router_w"].rearrange("(kc p) e -> p kc e", p=128), W=[b_rw])
        P.copy(rwh[:], rw[:], R=[b_rw], W=[b_rw])
        P.tt(rwl[:], rw[:], rwh[:], ALU.subtract, R=[b_rw], W=[b_rw])
        oaT = [sb(f"oaT{i}", [128, 8, 512], BF16) for i in range(2)]; obT = [sb(f"obT{i}", [128, 4, 512], BF16) for i in range(2)]
        mgt = [sb(f"mgt{i}", [128, 2, 512]) for i in range(3)]; b_mgt = P.bufs(3); xo = [sb(f"xo{i}", [128, 4, D]) for i in range(2)]
        b_in = P.bufs(2); b_xo = P.bufs(2)
        mT = sb("mT", [128, 8, 512], BF16); b_mT = P.buf()
        tA = [sb(f"tA{i}", [128, 512]) for i in range(2)]; tB = [sb(f"tB{i}", [128, 512]) for i in range(2)]; b_tA = P.bufs(2); b_tB = P.bufs(2)
        hT = sb("h2T", [128, 8, 512], BF16); b_hT = P.buf()
        hf = [sb(f"h2f{i}", [128, 512]) for i in range(2)]; b_hf = P.bufs(2); hl = sb("h2l", [128, 8, 512], BF16); b_hl = P.buf()
        ss = sb("ssd", [128, 4]); sd = sb("sdd", [128, 4]); rs = sb("rsd", [128, 4]); b_st = P.buf()
        junk = sb("junkd", [128, D], BF16); b_junk = P.buf()
        r1 = sb("r1", [128, 64]); r2 = sb("r2", [128, 64]); r3 = sb("r3", [128, 64]); r4 = sb("r4", [128, 64]); rg = sb("rg", [128, 40]); b_r = P.buf()
        oat_v = Sc["oat"].rearrange("h p t -> p h t"); obt_v = Sc["obt"].rearrange("h p t -> p h t")
        xo_v = I["xo"].rearrange("(c s r) d -> c r s d", s=4, r=128)
        x1_v = Sc["x1"].rearrange("(c s r) d -> c r s d", s=4, r=128); h2t_v = Sc["h2t"].rearrange("k p t -> p k t")
        rot = Rot(range(8))

        def load(j):
            s = j % 2
            qs = slice(j * 512, (j + 1) * 512)
            P.dma(oaT[s][:], oat_v[:, :, qs], W=[b_in[s]]); P.dma(obT[s][:], obt_v[:, :, qs], W=[b_in[s]])
            P.dma(xo[s][:], xo_v[j], W=[b_xo[s]])
        def stage1(j):
            s = j % 2
            for fc in range(8):
                mi = (j * 8 + fc) % 3
                P.dma(mgt[mi][:, 0, :], Sc["mg"][fc, :, j * 512:(j + 1) * 512], W=[b_mgt[mi]])
                P.dma(mgt[mi][:, 1, :], Sc["mg"][8 + fc, :, j * 512:(j + 1) * 512], W=[b_mgt[mi]])
                bA = rot.next()
                for hh in range(8):
                    P.mm(ps[:, bA, :], wda[:, hh, fc * 128:(fc + 1) * 128], oaT[s][:, hh, :], hh == 0, hh == 7, R=[b_w, b_in[s]], W=[pb[bA]])
                bB = rot.next()
                for pr in range(4):
                    P.mm(ps[:, bB, :], wnsa[:, pr, fc * 128:(fc + 1) * 128], obT[s][:, pr, :], pr == 0, pr == 3, R=[b_w, b_in[s]], W=[pb[bB]])
                q = fc % 2
                P.tt(tA[q][:], ps[:, bA, :], mgt[mi][:, 0, :], ALU.mult, R=[pb[bA], b_mgt[mi]], W=[b_tA[q]])
                P.tt(tB[q][:], ps[:, bB, :], mgt[mi][:, 1, :], ALU.mult, R=[pb[bB], b_mgt[mi]], W=[b_tB[q]])
                P.tt(mT[:, fc, :], tA[q][:], tB[q][:], ALU.add, R=[b_tA[q], b_tB[q]], W=[b_mT], eng="pool")
            for sub in range(4):
                for half in range(2):
                    bank = rot.next()
                    for fc in range(8):
                        P.mm(ps[:, bank, :], mT[:, fc, sub * 128:(sub + 1) * 128], wo[:, fc, half * 512:(half + 1) * 512], fc == 0, fc == 7, R=[b_mT, b_w], W=[pb[bank]])
                    P.tt(xo[s][:, sub, half * 512:(half + 1) * 512], ps[:, bank, :], xo[s][:, sub, half * 512:(half + 1) * 512], ALU.add, R=[pb[bank], b_xo[s]], W=[b_xo[s]])
            P.dma(x1_v[j], xo[s][:], R=[b_xo[s]])

        def stage2(j):
            s = j % 2
            norm_chunk(K, xo[s], b_xo[s], hT, b_hT, ss, sd, rs, b_st, junk, b_junk, K.a2, K.modT[:, 24:32], rot, hT_lo=(hf, b_hf, hl, b_hl))
            P.dma(h2t_v[:, :, j * 512:(j + 1) * 512], hT[:], R=[b_hT])
            for sub in range(4):
                tile = j * 4 + sub
                bank = rot.next()
                cs = slice(sub * 128, (sub + 1) * 128)
                n = 0
                for kc in range(8):
                    for (l, r, bl) in [(hT, rwh, b_hT), (hT, rwl, b_hT), (hl, rwh, b_hl)]:
                        P.mm(ps[:, bank, 0:64], l[:, kc, cs], r[:, kc, :], n == 0, n == 23, R=[bl, b_rw], W=[pb[bank]])
                        n += 1
                route(K, ps[:, bank, 0:64], pb[bank], tile, r1, r2, r3, r4, rg, b_r)

        load(0)
        load(1)
        stage1(0)
        for j in range(8):
            if j + 1 < 8:
                stage1(j + 1)
            stage2(j)
            if j + 2 < 8:
                load(j + 2)
        P.barrier()
        P.flush(K.block)


def route(K, logits, b_log, tile, r1, r2, r3, r4, rg, b_r):
    P = K.P
    R = [b_r]
    P.act(r1[:], logits, AF.Sigmoid, R=[b_log], W=[b_r])
    P.tt(r2[:], r1[:], K.rbbc[:], ALU.add, R=[b_r, K.b_rb], W=[b_r])
    r2g = r2[:].rearrange("p (g e) -> p g e", e=8)
    r3g = r3[:].rearrange("p (g e) -> p g e", e=8)
    P.op("dve", lambda e: e.tensor_reduce(out=rg[:, 0:8], in_=r2g, axis=AX.X, op=ALU.max), R, R)
    P.tt(r3g, r2g, rg[:, 0:8].unsqueeze(2).to_broadcast([128, 8, 8]), ALU.is_equal, R=R, W=R)
    P.stt(r3[:], r3[:], -1.0e9, r2[:], ALU.mult, ALU.add, R=R, W=R)
    P.op("dve", lambda e: e.tensor_reduce(out=rg[:, 8:16], in_=r3g, axis=AX.X, op=ALU.max), R, R)
    P.tt(rg[:, 16:24], rg[:, 0:8], rg[:, 8:16], ALU.add, R=R, W=R)
    P.op("dve", lambda e: e.max(out=rg[:, 24:32], in_=rg[:, 16:24]), R, R)
    P.ts(rg[:, 32:40], rg[:, 16:24], rg[:, 27:28], -1.0e30, ALU.is_lt, ALU.mult, R=R, W=R)
    P.tt(r3g, r2g, rg[:, 32:40].unsqueeze(2).to_broadcast([128, 8, 8]), ALU.add, R=R, W=R)
    P.op("dve", lambda e: e.max(out=rg[:, 0:8], in_=r3[:]), R, R)
    P.ts(r4[:], r3[:], rg[:, 7:8], None, ALU.is_ge, R=R, W=R)
    P.tt(r4[:], r4[:], r1[:], ALU.mult, R=R, W=R)
    P.op("dve", lambda e: e.tensor_reduce(out=rg[:, 8:9], in_=r4[:], axis=AX.X, op=ALU.add), R, R)
    P.op("dve", lambda e: e.reciprocal(out=rg[:, 9:10], in_=rg[:, 8:9]), R, R)
    P.ts(K.Wr[:, tile, :], r4[:], rg[:, 9:10], 2.5, ALU.mult, ALU.mult, R=R, W=[K.b_Wr])


def phase_moe(K, out_d):
    nc, P, I, Sc, ps, pb = K.nc, K.P, K.I, K.Sc, K.ps, K.pb
    with contextlib.ExitStack() as ph:
        sb = lambda n, s, d=F32: K.sb(n, s, d, ph)
        h2 = sb("h2m", [128, 8, 2048], BF16); b_h2 = P.buf()
        acc = sb("accm", [128, 16, D]); b_acc = P.bufs(16)
        NW = 3
        wg = [sb(f"wg{i}", [128, 8, 256], BF16) for i in range(NW)]; wu = [sb(f"wu{i}", [128, 8, 256], BF16) for i in range(NW)]
        wd = [sb(f"wd{i}", [128, 2, D], BF16) for i in range(NW)]; b_wt = P.bufs(NW)
        sg = [sb(f"sg{i}", [128, 512]) for i in range(2)]; b_sg = P.bufs(2)
        aT = [sb(f"aT{i}", [128, 512], BF16) for i in range(2)]; b_aT = P.bufs(2)
        x1t = [sb(f"x1t{i}", [128, D]) for i in range(2)]; b_x1 = P.bufs(2)
        ss = sb("ssm", [128, 2]); b_ss = P.buf()
        junk = sb("junkm", [128, D], BF16); b_junk = P.buf()
        h2t_v = Sc["h2t"].rearrange("k p t -> p k t")
        x1_v = Sc["x1"].rearrange("(t r) d -> t r d", r=128)
        out_v = out_d.rearrange("(t r) d -> t r d", r=128)
        grot = Rot([0, 1]); urot = Rot([2, 3]); drot = Rot([4, 5, 6, 7])
        order = [64] + list(range(64))
        outs = []

        def wload(idx):
            e = order[idx % 65]
            s = idx % NW
            if e == 64:
                g_ap, u_ap, d_ap = I["sg"], I["su"], I["sd"]
            else:
                g_ap, u_ap, d_ap = I["eg"][e], I["eu"][e], I["ed"][e]
            P.dma(wg[s][:], g_ap.rearrange("(kc p) h -> p kc h", p=128), W=[b_wt[s]], eng="pool")
            P.dma(wu[s][:], u_ap.rearrange("(kc p) h -> p kc h", p=128), W=[b_wt[s]], eng="pool")
            dv = d_ap.rearrange("(hc p) f -> p hc f", p=128)
            P.dma(wd[s][:, :, 0:512], dv[:, :, 0:512], W=[b_wt[s]], eng="pool")
            P.dma(wd[s][:, :, 512:1024], dv[:, :, 512:1024], W=[b_wt[s]], eng="pool")

        total = 2 * 65
        wload(0); wload(1)

        def gu(sc, ei, ch, s):
            bG = grot.next(); bU = urot.next()
            ts_ = slice(ch * 256, (ch + 1) * 256)
            for hc in range(2):
                for kc in range(8):
                    P.mm(ps[:, bG, hc * 256:(hc + 1) * 256], wg[s][:, kc, hc * 128:(hc + 1) * 128], h2[:, kc, ts_], kc == 0, kc == 7, R=[b_wt[s], b_h2], W=[pb[bG]])
            for hc in range(2):
                for kc in range(8):
                    P.mm(ps[:, bU, hc * 256:(hc + 1) * 256], wu[s][:, kc, hc * 128:(hc + 1) * 128], h2[:, kc, ts_], kc == 0, kc == 7, R=[b_wt[s], b_h2], W=[pb[bU]])
            return bG, bU

        def rest(sc, ei, ch, s, bG, bU, q):
            e = order[ei]
            P.act(sg[q][:], ps[:, bG, :], AF.Silu, R=[pb[bG]], W=[b_sg[q]])
            P.tt(aT[q][:], sg[q][:], ps[:, bU, :], ALU.mult, R=[b_sg[q], pb[bU]], W=[b_aT[q]])
            for sub in range(2):
                lt = ch * 2 + sub
                gt = sc * 16 + lt
                for half in range(2):
                    bank = drot.next()
                    for hc in range(2):
                        P.mm(ps[:, bank, :], aT[q][:, hc * 256 + sub * 128:hc * 256 + (sub + 1) * 128], wd[s][:, hc, half * 512:(half + 1) * 512], hc == 0, hc == 1, R=[b_aT[q], b_wt[s]], W=[pb[bank]])
                    dst = acc[:, lt, half * 512:(half + 1) * 512]
                    if ei == 0:
                        P.copy(dst, ps[:, bank, :], R=[pb[bank]], W=[b_acc[lt]])
                    else:
                        P.stt(dst, ps[:, bank, :], K.Wr[:, gt, e:e + 1], dst, ALU.mult, ALU.add, R=[pb[bank], K.b_Wr, b_acc[lt]], W=[b_acc[lt]])

        def epilogue_tile(sc, lt):
            gt = sc * 16 + lt
            q = lt % 2
            P.dma(x1t[q][:], x1_v[gt], W=[b_x1[q]])
            P.tt(acc[:, lt, :], acc[:, lt, :], K.g2bc[:], ALU.mult, R=[b_acc[lt], K.b_gbc], W=[b_acc[lt]])
            P.tt(acc[:, lt, :], acc[:, lt, :], x1t[q][:], ALU.add, R=[b_acc[lt], b_x1[q]], W=[b_acc[lt]])
            P.act(junk[:], acc[:, lt, :], AF.Square, R=[b_acc[lt]], W=[b_junk, b_ss], accum_out=ss[:, 0:1])
            P.act(ss[:, 1:2], ss[:, 0:1], AF.Sqrt, R=[b_ss, K.b_eps], W=[b_ss], scale=1.0 / D, bias=K.epsc[:, 0:1])
            P.op("dve", lambda e: e.reciprocal(out=ss[:, 1:2], in_=ss[:, 1:2]), [b_ss], [b_ss])
            P.stt(acc[:, lt, :], acc[:, lt, :], ss[:, 1:2], K.fnwbc[:], ALU.mult, ALU.mult, R=[b_acc[lt], b_ss, K.b_gbc], W=[b_acc[lt]])
            outs.append(P.dma(out_v[gt], acc[:, lt, :], R=[b_acc[lt]]))

        def rest_and_epilogue(sc, ei, ch, s_, bG, bU, q):
            rest(sc, ei, ch, s_, bG, bU, q)
            if ei == 64:
                epilogue_tile(sc, ch * 2)
                epilogue_tile(sc, ch * 2 + 1)

        idx = 0
        it = 0
        for sc in range(2):
            P.dma(h2[:, 0:4, :], h2t_v[:, 0:4, sc * 2048:(sc + 1) * 2048], W=[b_h2])
            P.dma(h2[:, 4:8, :], h2t_v[:, 4:8, sc * 2048:(sc + 1) * 2048], W=[b_h2])
            steps = []
            for ei in range(65):
                for ch in range(8):
                    steps.append((ei, ch))
            pend = None
            for si, (ei, ch) in enumerate(steps):
                if ch == 0:
                    cur_s = idx % NW
                    idx += 1
                if ch == 1 and idx + 1 < total:
                    wload(idx + 1)
                bG, bU = gu(sc, ei, ch, cur_s)
                if pend is not None:
                    rest_and_epilogue(*pend)
                pend = (sc, ei, ch, cur_s, bG, bU, it % 2)
                it += 1
            rest_and_epilogue(*pend)
        P.wait_all("sp", outs)
        P.barrier()
        P.flush(K.block)


def _own_rows(p):
    r = np.arange(128)
    return np.concatenate([(2 * i + p) * 128 + r for i in range(32)])


def _consts(p):
    bf = ml_dtypes.bfloat16
    r = np.arange(128)
    qpos = _own_rows(p).astype(np.float64)
    c = {}
    c["refda"] = np.stack([-SL_A[h] * qpos for h in range(8)]).astype(np.float32).astype(bf)
    c["refn"] = np.stack([-SL_B[h] * qpos for h in range(8)]).astype(np.float32).astype(bf)
    c["kposT"] = (np.arange(64)[None, :] * 128 + r[:, None]).astype(np.float32)
    cc = np.arange(512).reshape(4, 128).T
    c["cposT"] = np.where(cc < 511, 16.0 * cc + 31.0, -1.0e6).astype(np.float32)
    qrel = np.concatenate([128 * (2 * sub + p) + r for sub in range(4)])
    dm = np.zeros((128, 8, 512), np.float32)
    for kt in range(8):
        krel = 128 * kt + r
        dm[:, kt, :] = np.where(krel[:, None] <= qrel[None, :], 0.0, NEG)
    c["dmask"] = dm.astype(bf)
    wm = np.zeros((128, 12, 512), np.float32)
    for kw in range(12):
        krel = 128 * (kw - 4) + r
        d = qrel[None, :] - krel[:, None]
        wm[:, kw, :] = np.where((d >= 0) & (d < 512), 0.0, NEG)
    c["wmask"] = wm.astype(bf)
    cmk = np.zeros((128, 3, 512), np.float32)
    for i, off in enumerate([128, 0, 64]):
        relc = 16 * (r - off) + 31
        cmk[:, i, :] = np.where(qrel[None, :] >= relc[:, None], 0.0, NEG)
    c["cmask"] = cmk.astype(bf)
    t = qpos.reshape(32, 128).T.astype(np.int64)
    cur = t // 64
    n = np.arange(128)[None, None, :]
    forced = (n == 0) | (n == cur[:, :, None]) | (n == cur[:, :, None] - 1)
    causal = n <= cur[:, :, None]
    c["addmask"] = np.where(causal, 1.0e4 * forced, -1.0e30).astype(np.float32).astype(bf)
    k = np.arange(S)
    c["wind"] = (((k[None, :] // 64) % 32) == np.arange(32)[:, None]).astype(np.float32).astype(bf)
    ci = np.arange(512)
    c0 = ci[:, None] * 16
    s0 = np.arange(128)[None, :] * 64
    ov = np.minimum(c0 + 32, s0 + 64) - np.maximum(c0, s0)
    M = np.clip(ov, 0, None).astype(np.float32) / 32.0
    M[511, :] = 0.0
    c["c2s"] = np.ascontiguousarray(M.reshape(4, 128, 128).transpose(1, 0, 2)).astype(bf)
    return c


def make_in_maps(inputs):
    f = lambda a: np.ascontiguousarray(np.asarray(a, dtype=np.float32))
    x = f(inputs["x"]); cvec = f(inputs["c"])
    shared = {
        "ada_w": f(inputs["ada_w"][0]), "ada_bT": f(inputs["ada_b"][0].reshape(48, 128).T), "ada_b": f(inputs["ada_b"][0].reshape(1, -1)),
        "n1wT": f(inputs["norm1_w"][0].reshape(8, 128).T), "n2wT": f(inputs["norm2_w"][0].reshape(8, 128).T),
        "w_in": f(inputs["w_in"][0]),
        "lq1": f(inputs["da_lq1"][0].reshape(1, 64)), "lk1": f(inputs["da_lk1"][0].reshape(1, 64)),
        "lq2": f(inputs["da_lq2"][0].reshape(1, 64)), "lk2": f(inputs["da_lk2"][0].reshape(1, 64)),
        "subln": f(inputs["da_subln_w"][0].reshape(1, 128)),
        "ck_peT": f(inputs["cmp_k_pe"][0].T), "ck_w1": f(inputs["cmp_k_w1"][0]), "ck_w2": f(inputs["cmp_k_w2"][0]),
        "cv_peT": f(inputs["cmp_v_pe"][0].T), "cv_w1": f(inputs["cmp_v_w1"][0]), "cv_w2": f(inputs["cmp_v_w2"][0]),
        "w_da_out": f(inputs["w_da_out"][0]), "w_nsa_out": f(inputs["w_nsa_out"][0]), "w_o": f(inputs["w_o"][0]),
        "router_w": f(inputs["router_w"][0]), "router_b": f(inputs["router_b"][0].reshape(1, 64)),
        "eg": f(inputs["exp_w_gate"][0]), "eu": f(inputs["exp_w_up"][0]), "ed": f(inputs["exp_w_down"][0]),
        "sg": f(inputs["sh_w_gate"][0]), "su": f(inputs["sh_w_up"][0]), "sd": f(inputs["sh_w_down"][0]),
        "fnw": f(inputs["final_norm_w"].reshape(1, D)),
    }
    consts = [_consts(0), _consts(1)]
    maps = []
    for core in range(8):
        b, p = core // 2, core % 2
        m = dict(shared)
        m.update(consts[p])
        m["xn"] = x[b]
        m["xo"] = np.ascontiguousarray(x[b][_own_rows(p)])
        m["cT"] = np.ascontiguousarray(cvec[b].reshape(8, 128).T)
        maps.append(m)
    return maps


_NC_CACHE = {}


def kernel(**inputs):
    if "nc" not in _NC_CACHE:
        _NC_CACHE["nc"] = build_nc()
    nc = _NC_CACHE["nc"]
    maps = make_in_maps(inputs)
    res = run_bass_kernel_spmd(nc, maps, core_ids=list(range(8)))
    out = np.empty((4, S, D), np.float32)
    for core in range(8):
        b, p = core // 2, core % 2
        out[b][_own_rows(p)] = res.results[core]["out"]
    return out
```

```python
import contextlib
import os
import numpy as np
import ml_dtypes
import concourse.bass as bass
import concourse.mybir as mybir
from concourse.bass_utils import run_bass_kernel_spmd

F32 = mybir.dt.float32
BF16 = mybir.dt.bfloat16
AF = mybir.ActivationFunctionType
ALU = mybir.AluOpType
AX = mybir.AxisListType

S = 8192
D = 1024
NQ = 4096
NEG = -30000.0
ENGS = ["pe", "act", "dve", "pool", "sp"]


class Tok:
    __slots__ = ("sem", "val")

    def __init__(self, sem, val):
        self.sem = sem
        self.val = val


class Buf:
    __slots__ = ("w", "r")

    def __init__(self):
        self.w = None
        self.r = {}


class Rot:
    def __init__(self, items):
        self.items = list(items)
        self.i = 0

    def next(self):
        v = self.items[self.i]
        self.i = (self.i + 1) % len(self.items)
        return v


class Prog:
    def __init__(self, nc, stack, n_dma_sems=(32, 16)):
        self.nc = nc
        self.ops = {e: [] for e in ENGS}
        self.cnt = {e: 0 for e in ENGS}
        self.esem = {}
        for e in ["pe", "act", "dve", "pool"]:
            self.esem[e] = stack.enter_context(nc.semaphore("S_" + e))
        self.dpool = {}
        for e, n in zip(["sp", "pool"], n_dma_sems):
            sems = [stack.enter_context(nc.semaphore(f"D_{e}{i}")) for i in range(n)]
            self.dpool[e] = {"sems": sems, "uses": [0] * n, "next": 0, "last": [None] * n}
        self.waited = {e: {} for e in ENGS}
        self.nops = 0

    def buf(self):
        return Buf()

    def bufs(self, n):
        return [Buf() for _ in range(n)]

    def _need(self, eng, tok, waits):
        if tok is None:
            return
        if eng == "pe" and tok.sem is self.esem["pe"]:
            return
        key = id(tok.sem)
        if self.waited[eng].get(key, 0) >= tok.val:
            return
        cur = waits.get(key)
        if cur is None or cur[1] < tok.val:
            waits[key] = (tok.sem, tok.val)

    def op(self, eng, fn, reads=(), writes=(), dma=False):
        waits = {}
        for b in reads:
            self._need(eng, b.w, waits)
        for b in writes:
            self._need(eng, b.w, waits)
            for t in b.r.values():
                self._need(eng, t, waits)
        if dma:
            pool = self.dpool[eng]
            j = pool["next"]
            pool["next"] = (j + 1) % len(pool["sems"])
            self._need(eng, pool["last"][j], waits)
            pool["uses"][j] += 1
            tok = Tok(pool["sems"][j], 16 * pool["uses"][j])
            pool["last"][j] = tok
            inc = 16
        else:
            self.cnt[eng] += 1
            tok = Tok(self.esem[eng], self.cnt[eng])
            inc = 1
        for key, (sem, val) in waits.items():
            self.waited[eng][key] = val
        self.ops[eng].append((list(waits.values()), fn, tok, inc))
        self.nops += 1
        for b in reads:
            b.r[id(tok.sem)] = tok
        for b in writes:
            b.w = tok
            b.r = {}
        return tok

    def wait_all(self, eng, toks):
        waits = {}
        for t in toks:
            self._need(eng, t, waits)
        for key, (sem, val) in waits.items():
            self.waited[eng][key] = val
        if waits:
            self.ops[eng].append((list(waits.values()), None, None, 0))

    def barrier(self):
        toks = [Tok(self.esem[f], self.cnt[f]) for f in ["pe", "act", "dve", "pool"] if self.cnt[f] > 0]
        for pool in self.dpool.values():
            toks += [t for t in pool["last"] if t is not None]
        for e in ENGS:
            self.wait_all(e, toks)

    def flush(self, block):
        def mk(ename):
            ops = self.ops[ename]
            self.ops[ename] = []

            def body(e):
                for waits, fn, tok, inc in ops:
                    for sem, val in waits:
                        e.wait_ge(sem, val)
                    if fn is not None:
                        fn(e).then_inc(tok.sem, inc)
            return body
        block.tensor(mk("pe"))
        block.scalar(mk("act"))
        block.vector(mk("dve"))
        block.gpsimd(mk("pool"))
        block.sync(mk("sp"))

    def mm(self, out, lhsT, rhs, start, stop, R=(), W=()):
        return self.op("pe", lambda e: e.matmul(out, lhsT=lhsT, rhs=rhs, start=start, stop=stop), R, W)

    def tr(self, out, in_, ident, R=(), W=()):
        return self.op("pe", lambda e: e.transpose(out=out, in_=in_, identity=ident), R, W)

    def act(self, out, in_, func, R=(), W=(), bias=None, scale=None, accum_out=None):
        kw = {}
        if bias is not None:
            kw["bias"] = bias
        if scale is not None:
            kw["scale"] = scale
        if accum_out is not None:
            kw["accum_out"] = accum_out
        return self.op("act", lambda e: e.activation(out=out, in_=in_, func=func, **kw), R, W)

    def tt(self, out, in0, in1, op, R=(), W=(), eng="dve"):
        return self.op(eng, lambda e: e.tensor_tensor(out=out, in0=in0, in1=in1, op=op), R, W)

    def ts(self, out, in0, s1, s2, op0, op1=None, R=(), W=(), eng="dve"):
        if op1 is None:
            return self.op(eng, lambda e: e.tensor_scalar(out=out, in0=in0, scalar1=s1, scalar2=None, op0=op0), R, W)
        return self.op(eng, lambda e: e.tensor_scalar(out=out, in0=in0, scalar1=s1, scalar2=s2, op0=op0, op1=op1), R, W)

    def stt(self, out, in0, scalar, in1, op0, op1, R=(), W=()):
        return self.op("dve", lambda e: e.scalar_tensor_tensor(out=out, in0=in0, scalar=scalar, in1=in1, op0=op0, op1=op1), R, W)

    def copy(self, out, in_, R=(), W=(), eng="dve"):
        return self.op(eng, lambda e: e.tensor_copy(out=out, in_=in_), R, W)

    def memset(self, ap, val, W=(), eng="pool"):
        return self.op(eng, lambda e: e.memset(ap, val), (), W)

    def dma(self, out, in_, R=(), W=(), eng="sp"):
        return self.op(eng, lambda e: e.dma_start(out=out, in_=in_), R, W, dma=True)


class Ctx:
    pass


def _slopes():
    i = np.arange(1, 17, dtype=np.float64)
    s = 2.0 ** (-8.0 * i / 16.0)
    return s[0::2], s[1::2]


SL_A, SL_B = _slopes()
SKIP_THR = 48.0


def first_tile(j, m):
    t0 = 0
    while t0 < 8 * j and m * (1024 * j - (128 * t0 + 127)) > SKIP_THR:
        t0 += 1
    return t0

C_DAQ, C_DAK, C_DAV, C_NQ = 0, 1024, 2048, 3072
C_CK, C_CV, C_SK, C_SV, C_WK, C_WV = 3584, 3712, 3840, 3968, 4096, 4224
C_NG, C_MG = 4352, 4376


def build_nc(stop_after=None, debug=False):
    nc = bass.Bass("TRN2", target_bir_lowering=False)
    K = Ctx()
    K.nc = nc
    K.debug = debug

    def din(name, shape, dt=F32):
        return nc.dram_tensor(name, list(shape), dt, kind="ExternalInput").ap()

    def dscr(name, shape, dt=BF16):
        kind = "ExternalOutput" if debug else "Internal"
        return nc.dram_tensor(name, list(shape), dt, kind=kind).ap()

    I = {}
    I["xn"] = din("xn", [S, D]); I["xo"] = din("xo", [NQ, D]); I["cT"] = din("cT", [128, 8])
    I["ada_w"] = din("ada_w", [D, 6 * D]); I["ada_bT"] = din("ada_bT", [128, 48]); I["ada_b"] = din("ada_b", [1, 6 * D])
    I["n1wT"] = din("n1wT", [128, 8]); I["n2wT"] = din("n2wT", [128, 8])
    I["w_in"] = din("w_in", [D, 6424])
    for nm in ["lq1", "lk1", "lq2", "lk2"]:
        I[nm] = din(nm, [1, 64])
    I["subln"] = din("subln", [1, 128])
    I["ck_peT"] = din("ck_peT", [64, 32]); I["ck_w1"] = din("ck_w1", [2048, 128]); I["ck_w2"] = din("ck_w2", [128, 64])
    I["cv_peT"] = din("cv_peT", [64, 32]); I["cv_w1"] = din("cv_w1", [2048, 128]); I["cv_w2"] = din("cv_w2", [128, 64])
    I["w_da_out"] = din("w_da_out", [1024, D]); I["w_nsa_out"] = din("w_nsa_out", [512, D]); I["w_o"] = din("w_o", [D, D])
    I["router_w"] = din("router_w", [D, 64]); I["router_b"] = din("router_b", [1, 64])
    I["eg"] = din("eg", [64, D, 256]); I["eu"] = din("eu", [64, D, 256]); I["ed"] = din("ed", [64, 256, D])
    I["sg"] = din("sg", [D, 256]); I["su"] = din("su", [D, 256]); I["sd"] = din("sd", [256, D])
    I["fnw"] = din("fnw", [1, D])
    I["refda"] = din("refda", [8, NQ], BF16); I["refn"] = din("refn", [8, NQ], BF16); I["kposT"] = din("kposT", [128, 64]); I["cposT"] = din("cposT", [128, 4])
    I["dmask"] = din("dmask", [128, 8, 512], BF16); I["wmask"] = din("wmask", [128, 12, 512], BF16)
    I["cmask"] = din("cmask", [128, 3, 512], BF16); I["addmask"] = din("addmask", [128, 32, 128], BF16)
    I["wind"] = din("wind", [32, S], BF16); I["c2s"] = din("c2s", [128, 4, 128], BF16)
    out_d = nc.dram_tensor("out", [NQ, D], F32, kind="ExternalOutput").ap()
    K.I = I

    Sc = {}
    Sc["ktda"] = dscr("s_ktda", [8, 128, S]); Sc["vda"] = dscr("s_vda", [S, 1024])
    Sc["nfm"] = dscr("s_nfm", [4, 128, S]); Sc["ntm"] = dscr("s_ntm", [S, 256])
    Sc["qtda"] = dscr("s_qtda", [8, 128, NQ]); Sc["qtn"] = dscr("s_qtn", [4, 128, NQ]); Sc["mg"] = dscr("s_mg", [16, 128, NQ], F32)
    Sc["oat"] = dscr("s_oat", [8, 128, NQ]); Sc["obt"] = dscr("s_obt", [4, 128, NQ])
    Sc["x1"] = dscr("s_x1", [NQ, D], F32); Sc["h2t"] = dscr("s_h2t", [8, 128, NQ])
    K.Sc = Sc
    if debug:
        K.dbg_kc = [dscr(f"dbg_kc{g}", [64, 512]) for g in range(2)]
        K.dbg_vc = [dscr(f"dbg_vc{g}", [128, 4, 64]) for g in range(2)]
        K.dbg_mod = dscr("dbg_mod", [128, 48], F32)
        K.dbg_bc = dscr("dbg_bc", [128, 3, D], F32)
        K.dbg_gates = dscr("dbg_gates", [128, 32, 24], F32)
        K.dbg_wr = dscr("dbg_wr", [128, 32, 64], F32)

    with contextlib.ExitStack() as st:
        P = Prog(nc, st)
        K.P = P
        block = st.enter_context(nc.Block())
        K.block = block
        ps = st.enter_context(nc.psum_tensor("ps", [128, 8, 512], F32))
        K.ps = ps
        K.pb = P.bufs(8)

        def sb(name, shape, dt=F32, stack=st):
            return stack.enter_context(nc.sbuf_tensor(name, list(shape), dt))
        K.sb = sb

        K.ident = sb("ident", [128, 128]); K.b_ident = P.buf()
        K.identb = sb("identb", [128, 128], BF16)
        K.modT = sb("modT", [128, 48]); K.b_modT = P.buf()
        K.a1 = sb("a1", [128, 8]); K.a2 = sb("a2", [128, 8]); K.b_a = P.buf()
        K.g1bc = sb("g1bc", [128, D]); K.g2bc = sb("g2bc", [128, D]); K.fnwbc = sb("fnwbc", [128, D]); K.b_gbc = P.buf()
        K.nlam = sb("nlam", [128, 1]); K.b_lam = P.buf()
        K.swbc = sb("swbc", [128, 128]); K.b_sw = P.buf()
        K.gates = sb("gates", [128, 32, 24]); K.b_gates = P.buf()
        K.Wr = sb("Wr", [128, 32, 64]); K.b_Wr = P.buf()
        K.rbbc = sb("rbbc", [128, 64]); K.b_rb = P.buf()
        K.kposT = sb("kposTs", [128, 64]); K.b_pos = P.buf()
        K.epsc = sb("epsc", [128, 2]); K.b_eps = P.buf()

        P.memset(K.ident[:], 0.0, W=[K.b_ident])
        P.op("pool", lambda e: e.affine_select(out=K.ident[:], in_=K.ident[:], pattern=[[-1, 128]], compare_op=ALU.not_equal, fill=1.0, base=0, channel_multiplier=1), [K.b_ident], [K.b_ident])
        P.copy(K.identb[:], K.ident[:], R=[K.b_ident], W=[K.b_ident])
        P.memset(K.epsc[:, 0:1], 1e-6, W=[K.b_eps]); P.memset(K.epsc[:, 1:2], 1e-5, W=[K.b_eps])
        P.dma(K.kposT[:], I["kposT"], W=[K.b_pos])
        P.dma(K.rbbc[:], bass.AP(I["router_b"].tensor, 0, [[0, 128], [1, 64]]), W=[K.b_rb])
        P.dma(K.fnwbc[:], bass.AP(I["fnw"].tensor, 0, [[0, 128], [1, D]]), W=[K.b_gbc])

        pw = st.enter_context(contextlib.ExitStack())
        K.projw = {}
        w_v = I["w_in"].rearrange("(kc p) w -> p kc w", p=128)
        for tag, srcs in [("kv", [(C_DAK, 1024), (C_DAV, 1024), (C_CK, 128), (C_CV, 128), (C_SK, 128), (C_WK, 128), (C_SV, 128), (C_WV, 128)]),
                          ("q", [(C_DAQ, 1024), (C_NQ, 512), (C_MG, 2048), (C_NG, 24)])]:
            WT = sum(wd for _, wd in srcs)
            wt = sb("w_" + tag, [128, 8, WT], BF16, pw); bw = P.buf()
            off = 0
            for c0, wd in srcs:
                for s0 in range(0, wd, 512):
                    s1 = min(wd, s0 + 512)
                    P.dma(wt[:, :, off + s0:off + s1], w_v[:, :, c0 + s0:c0 + s1], W=[bw], eng="pool")
                off += wd
            K.projw[tag] = (wt, bw)
        phase0(K)
        if debug:
            P.dma(K.dbg_mod, K.modT[:], R=[K.b_modT])
            P.dma(K.dbg_bc[:, 0, :], K.g1bc[:], R=[K.b_gbc]); P.dma(K.dbg_bc[:, 1, :], K.g2bc[:], R=[K.b_gbc]); P.dma(K.dbg_bc[:, 2, :], K.fnwbc[:], R=[K.b_gbc])
        P.barrier(); P.flush(block)
        done = stop_after == "0"
        if not done:
            proj_phase(K, kv=True)
            P.barrier(); P.flush(block)
            proj_phase(K, kv=False)
            if debug:
                P.dma(K.dbg_gates, K.gates[:], R=[K.b_gates])
            P.barrier(); P.flush(block)
            done = stop_after == "A"
        pw.close()
        if not done:
            phase_da(K)
            P.barrier(); P.flush(block)
            done = stop_after == "B"
        if not done:
            phase_nsa(K)
            P.barrier(); P.flush(block)
            done = stop_after == "C"
        if not done:
            phase_d(K)
            if debug:
                P.dma(K.dbg_wr, K.Wr[:], R=[K.b_Wr])
            P.barrier(); P.flush(block)
            done = stop_after == "D"
        if not done:
            phase_moe(K, out_d)
        else:
            z = sb("zdbg", [128, D])
            bz = P.buf()
            P.memset(z[:], 0.0, W=[bz])
            P.dma(out_d[0:128, :], z[:], R=[bz])
        P.barrier()
        P.flush(block)
    return nc


def phase0(K):
    nc, P, I, ps, pb = K.nc, K.P, K.I, K.ps, K.pb
    with contextlib.ExitStack() as ph:
        sb = lambda n, s, d=F32: K.sb(n, s, d, ph)
        NAD = 3
        adaw = [sb(f"adaw{i}", [128, 8, 512]) for i in range(NAD)]; b_adaw = P.bufs(NAD)
        cTt = sb("cTt", [128, 8]); cact = sb("cact", [128, 8]); b_c = P.buf()
        cb = sb("cb", [128, 8, 128]); b_cb = P.buf()
        ones = sb("ones0", [128, 128]); b_ones = P.buf()
        adabT = sb("adabT", [128, 48]); n1wT = sb("n1wT_s", [128, 8]); n2wT = sb("n2wT_s", [128, 8]); b_small = P.buf()
        adabbc = sb("adabbc", [128, 2, D]); b_abc = P.buf()
        lv = sb("lv", [128, 4, 64]); lt = sb("lt", [128, 2, 64]); ls = sb("ls", [128, 2]); le = sb("le", [128, 2]); b_l = P.buf()
        swt = sb("swt", [128, 128])

        P.dma(cTt[:], I["cT"], W=[b_c])
        P.dma(adabT[:], I["ada_bT"], W=[b_small]); P.dma(n1wT[:], I["n1wT"], W=[b_small]); P.dma(n2wT[:], I["n2wT"], W=[b_small])
        P.dma(adabbc[:, 0, :], bass.AP(I["ada_b"].tensor, 2 * D, [[0, 128], [1, D]]), W=[b_abc])
        P.dma(adabbc[:, 1, :], bass.AP(I["ada_b"].tensor, 5 * D, [[0, 128], [1, D]]), W=[b_abc])
        for i, nm in enumerate(["lq1", "lk1", "lq2", "lk2"]):
            P.dma(lv[:, i, :], bass.AP(I[nm].tensor, 0, [[0, 128], [1, 64]]), W=[b_l])
        P.dma(swt[:], bass.AP(I["subln"].tensor, 0, [[0, 128], [1, 128]]), W=[K.b_sw])
        P.ts(K.swbc[:], swt[:], 0.8, None, ALU.mult, R=[K.b_sw], W=[K.b_sw])
        P.tt(lt[:, 0, :], lv[:, 0, :], lv[:, 1, :], ALU.mult, R=[b_l], W=[b_l])
        P.tt(lt[:, 1, :], lv[:, 2, :], lv[:, 3, :], ALU.mult, R=[b_l], W=[b_l])
        P.op("dve", lambda e: e.tensor_reduce(out=ls[:], in_=lt[:], axis=AX.X, op=ALU.add), [b_l], [b_l])
        P.act(le[:], ls[:], AF.Exp, R=[b_l], W=[b_l])
        P.tt(K.nlam[:], le[:, 1:2], le[:, 0:1], ALU.subtract, R=[b_l], W=[K.b_lam])
        P.ts(K.nlam[:], K.nlam[:], -0.2, None, ALU.add, R=[K.b_lam], W=[K.b_lam])
        P.act(cact[:], cTt[:], AF.Silu, R=[b_c], W=[b_c])
        P.memset(ones[:], 1.0, W=[b_ones])
        for kc in range(8):
            P.act(cb[:, kc, :], ones[:], AF.Identity, R=[b_ones, b_c], W=[b_cb], scale=cact[:, kc:kc + 1])
        adaw_v = I["ada_w"].rearrange("(kc p) w -> p kc w", p=128)
        rot = Rot(range(8))
        for blk in range(12):
            sl = blk % NAD
            P.dma(adaw[sl][:], adaw_v[:, :, blk * 512:(blk + 1) * 512], W=[b_adaw[sl]])
            bank = rot.next()
            if blk in (4, 5, 10, 11):
                for kc in range(8):
                    P.mm(ps[:, bank, :], cb[:, kc, :], adaw[sl][:, kc, :], kc == 0, kc == 7, R=[b_cb, b_adaw[sl]], W=[pb[bank]])
                which = 0 if blk < 6 else 1
                half = blk % 2
                dst = (K.g1bc if which == 0 else K.g2bc)[:, half * 512:(half + 1) * 512]
                P.tt(dst, ps[:, bank, :], adabbc[:, which, half * 512:(half + 1) * 512], ALU.add, R=[pb[bank], b_abc], W=[K.b_gbc])
            else:
                for cc in range(4):
                    for kc in range(8):
                        P.mm(ps[:, bank, cc:cc + 1], adaw[sl][:, kc, cc * 128:(cc + 1) * 128], cact[:, kc:kc + 1], kc == 0, kc == 7, R=[b_c, b_adaw[sl]], W=[pb[bank]])
                P.tt(K.modT[:, blk * 4:(blk + 1) * 4], ps[:, bank, 0:4], adabT[:, blk * 4:(blk + 1) * 4], ALU.add, R=[pb[bank], b_small], W=[K.b_modT])
        P.stt(K.a1[:], K.modT[:, 8:16], 1.0, n1wT[:], ALU.add, ALU.mult, R=[K.b_modT, b_small], W=[K.b_a])
        P.stt(K.a2[:], K.modT[:, 32:40], 1.0, n2wT[:], ALU.add, ALU.mult, R=[K.b_modT, b_small], W=[K.b_a])
        P.barrier()
        P.flush(K.block)


def norm_chunk(K, xt, b_xt, hT, b_hT, ss, sd, rs, b_st, junk, b_junk, a_ap, sh_ap, rot, hT_lo=None):
    P, ps, pb = K.P, K.ps, K.pb
    for sub in range(4):
        P.act(junk[:], xt[:, sub, :], AF.Square, R=[b_xt], W=[b_junk, b_st], accum_out=ss[:, sub:sub + 1])
    P.act(sd[:], ss[:], AF.Sqrt, R=[b_st, K.b_eps], W=[b_st], scale=1.0 / D, bias=K.epsc[:, 0:1])
    P.op("dve", lambda e: e.reciprocal(out=rs[:], in_=sd[:]), [b_st], [b_st])
    for sub in range(4):
        P.ts(xt[:, sub, :], xt[:, sub, :], rs[:, sub:sub + 1], None, ALU.mult, R=[b_st, b_xt], W=[b_xt])
    for fc in range(8):
        bank = rot.next()
        for sub in range(4):
            P.tr(ps[:, bank, sub * 128:(sub + 1) * 128], xt[:, sub, fc * 128:(fc + 1) * 128], K.ident[:], R=[b_xt, K.b_ident], W=[pb[bank]])
        if hT_lo is None:
            P.act(hT[:, fc, :], ps[:, bank, :], AF.Identity, R=[pb[bank], K.b_a, K.b_modT], W=[b_hT], scale=a_ap[:, fc:fc + 1], bias=sh_ap[:, fc:fc + 1])
        else:
            hfl, b_hfl, lo, b_lo = hT_lo
            hf = hfl[fc % 2]; b_hf = b_hfl[fc % 2]
            P.act(hf[:], ps[:, bank, :], AF.Identity, R=[pb[bank], K.b_a, K.b_modT], W=[b_hf], scale=a_ap[:, fc:fc + 1], bias=sh_ap[:, fc:fc + 1])
            P.copy(hT[:, fc, :], hf[:], R=[b_hf], W=[b_hT])
            P.tt(lo[:, fc, :], hf[:], hT[:, fc, :], ALU.subtract, R=[b_hf, b_hT], W=[b_lo])


def proj_phase(K, kv):
    nc, P, I, Sc, ps, pb = K.nc, K.P, K.I, K.Sc, K.ps, K.pb
    with contextlib.ExitStack() as ph:
        sb = lambda n, s, d=F32: K.sb(n, s, d, ph)
        tag = "kv" if kv else "q"
        if kv:
            srcs = [(C_DAK, 1024), (C_DAV, 1024), (C_CK, 128), (C_CV, 128), (C_SK, 128), (C_WK, 128), (C_SV, 128), (C_WV, 128)]
            x_d, nch = I["xn"], 16
        else:
            srcs = [(C_DAQ, 1024), (C_NQ, 512), (C_MG, 2048), (C_NG, 24)]
            x_d, nch = I["xo"], 8
        w, b_w = K.projw[tag]
        xt = [sb(f"xt{i}_" + tag, [128, 4, D]) for i in range(2)]; b_xt = P.bufs(2)
        hT = [sb(f"hT{i}_" + tag, [128, 8, 512], BF16) for i in range(2)]; b_hT = P.bufs(2)
        ss = [sb(f"ss{i}_" + tag, [128, 4]) for i in range(2)]; sd = [sb(f"sd{i}_" + tag, [128, 4]) for i in range(2)]
        rs = [sb(f"rs{i}_" + tag, [128, 4]) for i in range(2)]; b_st = P.bufs(2)
        junk = sb("junk_" + tag, [128, D], BF16); b_junk = P.buf()
        NST = 6
        stg = [sb(f"stg{i}_" + tag, [128, 512], BF16) for i in range(NST)]; b_stg = P.bufs(NST)
        srot = Rot(range(NST))
        if not kv:
            stgf = [sb(f"stgf{i}", [128, 512]) for i in range(4)]; b_stgf = P.bufs(4)
            frot = Rot(range(4))
        rot = Rot(range(8))
        x_v = x_d.rearrange("(c s r) d -> c r s d", s=4, r=128)

        def load(c):
            P.dma(xt[c % 2][:], x_v[c], W=[b_xt[c % 2]])
        load(0)
        evq = Rot(["dve", "act"])
        for c in range(nch):
            if c + 1 < nch:
                load(c + 1)
            s = c % 2
            norm_chunk(K, xt[s], b_xt[s], hT[s], b_hT[s], ss[s], sd[s], rs[s], b_st[s], junk, b_junk, K.a1, K.modT[:, 0:8], rot)
            h = hT[s]
            tok0 = c * 512

            def fm_group(col, dst_ap, mode):
                bank = rot.next()
                for kc in range(8):
                    P.mm(ps[:, bank, :], w[:, kc, col:col + 128], h[:, kc, :], kc == 0, kc == 7, R=[b_w, b_hT[s]], W=[pb[bank]])
                if mode == "sig":
                    j = frot.next()
                    P.act(stgf[j][:], ps[:, bank, :], AF.Sigmoid, R=[pb[bank]], W=[b_stgf[j]])
                    P.dma(dst_ap, stgf[j][:], R=[b_stgf[j]])
                else:
                    j = srot.next()
                    eng = evq.next()
                    if mode == "q":
                        if eng == "act":
                            P.act(stg[j][:], ps[:, bank, :], AF.Copy, R=[pb[bank]], W=[b_stg[j]], scale=0.125)
                        else:
                            P.ts(stg[j][:], ps[:, bank, :], 0.125, None, ALU.mult, R=[pb[bank]], W=[b_stg[j]])
                    else:
                        if eng == "act":
                            P.act(stg[j][:], ps[:, bank, :], AF.Copy, R=[pb[bank]], W=[b_stg[j]])
                        else:
                            P.copy(stg[j][:], ps[:, bank, :], R=[pb[bank]], W=[b_stg[j]])
                    P.dma(dst_ap, stg[j][:], R=[b_stg[j]])

            if kv:
                for hh in range(8):
                    fm_group(hh * 128, Sc["ktda"][hh, :, tok0:tok0 + 512], "k")
                for gi in range(4):
                    fm_group(2048 + gi * 128, Sc["nfm"][gi, :, tok0:tok0 + 512], "k")
                for sub in range(4):
                    for (col, wd, dst) in [(1024, 512, Sc["vda"][tok0 + sub * 128:tok0 + (sub + 1) * 128, 0:512]),
                                           (1536, 512, Sc["vda"][tok0 + sub * 128:tok0 + (sub + 1) * 128, 512:1024]),
                                           (2560, 256, Sc["ntm"][tok0 + sub * 128:tok0 + (sub + 1) * 128, :])]:
                        bank = rot.next()
                        for kc in range(8):
                            P.mm(ps[:, bank, 0:wd], h[:, kc, sub * 128:(sub + 1) * 128], w[:, kc, col:col + wd], kc == 0, kc == 7, R=[b_w, b_hT[s]], W=[pb[bank]])
                        j = srot.next()
                        eng = evq.next()
                        if eng == "act":
                            P.act(stg[j][:, 0:wd], ps[:, bank, 0:wd], AF.Copy, R=[pb[bank]], W=[b_stg[j]])
                        else:
                            P.copy(stg[j][:, 0:wd], ps[:, bank, 0:wd], R=[pb[bank]], W=[b_stg[j]])
                        P.dma(dst, stg[j][:, 0:wd], R=[b_stg[j]])
            else:
                for hh in range(8):
                    fm_group(hh * 128, Sc["qtda"][hh, :, tok0:tok0 + 512], "q")
                for pr in range(4):
                    fm_group(1024 + pr * 128, Sc["qtn"][pr, :, tok0:tok0 + 512], "q")
                for fc in range(16):
                    fm_group(1536 + fc * 128, Sc["mg"][fc, :, tok0:tok0 + 512], "sig")
                for sub in range(4):
                    bank = rot.next()
                    for kc in range(8):
                        P.mm(ps[:, bank, 0:24], h[:, kc, sub * 128:(sub + 1) * 128], w[:, kc, 3584:3608], kc == 0, kc == 7, R=[b_w, b_hT[s]], W=[pb[bank]])
                    P.act(K.gates[:, c * 4 + sub, :], ps[:, bank, 0:24], AF.Sigmoid, R=[pb[bank]], W=[K.b_gates])
        P.barrier()
        P.flush(K.block)


class Job:
    __slots__ = ("kt", "qt", "masks", "bias", "v", "W", "acc", "first", "last", "RS", "RB", "RV", "epi", "pre", "subs", "fs", "ls")


def run_attn(K, jobs, PT, b_PT, sbanks):
    P, ps, pb = K.P, K.ps, K.pb
    n = len(jobs)
    LA = len(sbanks) - 1
    srot = Rot(sbanks)
    prot = Rot(range(len(PT)))
    slot_of = {}

    def issue_S(i):
        jb = jobs[i]
        if jb.pre is not None:
            jb.pre()
        sbk = srot.next()
        c0 = 128 * min(jb.subs)
        c1 = 128 * (max(jb.subs) + 1)
        P.mm(ps[:, sbk, c0:c1], jb.kt, jb.qt[:, c0:c1], True, len(jb.masks) == 0, R=jb.RS, W=[pb[sbk]])
        for mi, (ml, mr, mR) in enumerate(jb.masks):
            P.mm(ps[:, sbk, c0:c1], ml, mr[:, c0:c1], False, mi == len(jb.masks) - 1, R=mR, W=[pb[sbk]])
        sl = prot.next()
        slot_of[i] = sl
        P.act(PT[sl][:, c0:c1], ps[:, sbk, c0:c1], AF.Exp, R=[pb[sbk]] + jb.RB, W=[b_PT[sl]], bias=jb.bias, scale=1.0)

    def issue_PV(i):
        jb = jobs[i]
        sl = slot_of.pop(i)
        for sub in jb.subs:
            f = jb.first if jb.fs is None else jb.fs[sub]
            l = jb.last if jb.ls is None else jb.ls[sub]
            P.mm(ps[:, jb.acc[sub], 0:jb.W], PT[sl][:, sub * 128:(sub + 1) * 128], jb.v, f, l, R=[b_PT[sl]] + jb.RV, W=[pb[jb.acc[sub]]])
        if jb.epi is not None:
            jb.epi()

    for step in range(n + LA):
        if step < n:
            issue_S(step)
        if step >= LA:
            issue_PV(step - LA)


def band_subs(kt, m, window=False):
    out = []
    for i in range(4):
        if 2 * i + 1 < kt:
            continue
        if m * max(0, 128 * (2 * i - kt) - 127) > SKIP_THR:
            continue
        if window and 2 * i - kt >= 5:
            continue
        out.append(i)
    return out


def assign_flags(group):
    first = {}
    last = {}
    for idx, jb in enumerate(group):
        for sub in jb.subs:
            first.setdefault(sub, idx)
            last[sub] = idx
    assert sorted(first) == [0, 1, 2, 3]
    for idx, jb in enumerate(group):
        jb.fs = {sub: first[sub] == idx for sub in jb.subs}
        jb.ls = {sub: last[sub] == idx for sub in jb.subs}


def mkjob(kt, qt, masks, bias, v, W, acc, first, last, RS, RB, RV, epi=None):
    j = Job()
    j.kt, j.qt, j.masks, j.bias, j.v, j.W, j.acc = kt, qt, masks, bias, v, W, acc
    j.first, j.last, j.RS, j.RB, j.RV, j.epi = first, last, RS, RB, RV, epi
    j.pre = None
    j.subs = [0, 1, 2, 3]
    j.fs = None
    j.ls = None
    return j


def phase_da(K):
    nc, P, I, Sc, ps, pb = K.nc, K.P, K.I, K.Sc, K.ps, K.pb
    with contextlib.ExitStack() as ph:
        sb = lambda n, s, d=F32: K.sb(n, s, d, ph)
        KT = [[sb(f"KT{c}{i}", [128, S], BF16) for i in range(2)] for c in range(2)]
        b_KT = [[P.buf() for i in range(2)] for c in range(2)]
        QT = [[sb(f"QT{c}{i}", [128, NQ], BF16) for i in range(2)] for c in range(2)]
        b_QT = [[P.buf() for i in range(2)] for c in range(2)]
        V = [sb(f"Vd{i}", [128, 64, 129], BF16) for i in range(2)]; b_V = P.bufs(2)
        bK = [sb(f"bK{i}", [128, 64]) for i in range(2)]; b_bK = P.bufs(2)
        dm = sb("dmask_s", [128, 8, 512], BF16); b_dm = P.buf()
        PT = [sb(f"PT{i}", [128, 512], BF16) for i in range(6)]; b_PT = P.bufs(6)
        o1 = sb("o1", [128, 4, 128]); b_o1 = P.buf()
        oo = [sb(f"oo{i}", [128, 4, 128]) for i in range(2)]; b_oo = P.bufs(2)
        on = [sb(f"on{i}", [128, 4, 128]) for i in range(2)]; b_on = P.bufs(2)
        rz = [sb(f"rz{i}", [128, 8]) for i in range(2)]; b_rz = P.bufs(2)
        ssq = [sb(f"ssq{i}", [128, 12]) for i in range(2)]; b_ssq = P.bufs(2)
        jk = sb("jk", [128, 128]); b_jk = P.buf()
        nh = sb("nh", [128, 4]); b_nh = P.buf()
        ost = [sb(f"ost{i}", [128, 512], BF16) for i in range(2)]; b_ost = P.bufs(2)
        P.memset(nh[:], -0.5, W=[b_nh])
        P.dma(dm[:], I["dmask"], W=[b_dm])
        for c in range(2):
            for i in range(2):
                P.memset(KT[c][i][64:65, :], 1.0, W=[b_KT[c][i]])
        for i in range(2):
            P.memset(V[i][:, :, 128:129], 1.0, W=[b_V[i]], eng="dve")
        vda_v = Sc["vda"].rearrange("(n r) d -> r n d", r=128)

        def load(hh):
            s = hh % 2
            m = float(SL_A[hh])
            for c in range(2):
                P.dma(KT[c][s][0:64, :], Sc["ktda"][hh, c * 64:(c + 1) * 64, :], W=[b_KT[c][s]])
                P.dma(QT[c][s][0:64, :], Sc["qtda"][hh, c * 64:(c + 1) * 64, :], W=[b_QT[c][s]])
                P.dma(QT[c][s][64:65, :], I["refda"][hh:hh + 1, :], W=[b_QT[c][s]])
            P.dma(V[s][:, :, 0:128], vda_v[:, :, hh * 128:(hh + 1) * 128], W=[b_V[s]])
            P.ts(bK[s][:], K.kposT[:], m, None, ALU.mult, R=[K.b_pos], W=[b_bK[s]], eng="pool")

        accb = [3, 4, 5, 6]
        TB = 7
        state = {"k": 0, "deferred": None, "e": 0, "short": False}
        ev = [sb(f"evd{i}", [128, 4, 129]) for i in range(3)]; b_ev = P.bufs(3)

        def make_epi(hh, j, comp, short):
            s = hh % 2

            def epi():
                state["short"] = short
                k = state["k"] % 2
                x = state["e"] % 3
                state["e"] += 1
                for sub in range(4):
                    if state["short"]:
                        P.act(ev[x][:, sub, :], ps[:, accb[sub], 0:129], AF.Copy, R=[pb[accb[sub]]], W=[b_ev[x]])
                    else:
                        P.copy(ev[x][:, sub, :], ps[:, accb[sub], 0:129], R=[pb[accb[sub]]], W=[b_ev[x]])
                if comp == 0:
                    for sub in range(4):
                        P.op("dve", lambda e, sub=sub: e.reciprocal(out=rz[k][:, sub:sub + 1], in_=ev[x][:, sub, 128:129]), [b_ev[x]], [b_rz[k]])
                        P.ts(o1[:, sub, :], ev[x][:, sub, 0:128], rz[k][:, sub:sub + 1], None, ALU.mult, R=[b_ev[x], b_rz[k]], W=[b_o1])
                    return
                for sub in range(4):
                    P.op("dve", lambda e, sub=sub: e.reciprocal(out=rz[k][:, 4 + sub:5 + sub], in_=ev[x][:, sub, 128:129]), [b_ev[x]], [b_rz[k]])
                    P.tt(rz[k][:, 4 + sub:5 + sub], rz[k][:, 4 + sub:5 + sub], K.nlam[:], ALU.mult, R=[b_rz[k], K.b_lam], W=[b_rz[k]])
                    P.stt(oo[k][:, sub, :], ev[x][:, sub, 0:128], rz[k][:, 4 + sub:5 + sub], o1[:, sub, :], ALU.mult, ALU.add, R=[b_ev[x], b_rz[k], b_o1], W=[b_oo[k]])
                for sub in range(4):
                    P.op("dve", lambda e, sub=sub: e.scalar_tensor_tensor(out=jk[:], in0=oo[k][:, sub, :], scalar=1.0, in1=oo[k][:, sub, :], op0=ALU.mult, op1=ALU.mult, accum_out=ssq[k][:, sub:sub + 1]), [b_oo[k]], [b_jk, b_ssq[k]])
                P.ts(ssq[k][:, 4:8], ssq[k][:, 0:4], 1.0 / 128.0, 1e-5, ALU.mult, ALU.add, R=[b_ssq[k]], W=[b_ssq[k]])
                P.tt(ssq[k][:, 8:12], ssq[k][:, 4:8], nh[:], ALU.pow, R=[b_ssq[k], b_nh], W=[b_ssq[k]], eng="pool")
                for sub in range(4):
                    P.stt(on[k][:, sub, :], oo[k][:, sub, :], ssq[k][:, 8 + sub:9 + sub], K.swbc[:], ALU.mult, ALU.mult, R=[b_oo[k], b_ssq[k], K.b_sw], W=[b_on[k]])

                def deferred(k=k, hh=hh, j=j):
                    for sub in range(4):
                        P.tr(ps[:, TB, sub * 128:(sub + 1) * 128], on[k][:, sub, :], K.ident[:], R=[b_on[k], K.b_ident], W=[pb[TB]])
                    P.copy(ost[k][:], ps[:, TB, :], R=[pb[TB]], W=[b_ost[k]])
                    P.dma(Sc["oat"][hh, :, j * 512:(j + 1) * 512], ost[k][:], R=[b_ost[k]])
                prev = state["deferred"]
                state["deferred"] = deferred
                state["k"] += 1
                if prev is not None:
                    prev()
            return epi

        load(0)
        for hh in range(8):
            if hh + 1 < 8:
                load(hh + 1)
            s = hh % 2
            jobs = []
            for j in range(8):
                ntile = 8 * j + 8
                tf = first_tile(j, float(SL_A[hh]))
                for comp in range(2):
                    grp = []
                    for t in range(0, ntile):
                        subs = band_subs(t - 8 * j, float(SL_A[hh]))
                        if not subs:
                            continue
                        masks = []
                        if t >= 8 * j:
                            masks = [(K.identb[:], dm[:, t - 8 * j, :], [K.b_ident, b_dm])]
                        jb = mkjob(KT[comp][s][0:65, t * 128:(t + 1) * 128], QT[comp][s][0:65, j * 512:(j + 1) * 512], masks,
                                   bK[s][:, t:t + 1], V[s][:, t, :], 129, accb, t == tf, t == ntile - 1,
                                   [b_KT[comp][s], b_QT[comp][s]], [b_bK[s]], [b_V[s]],
                                   None)
                        jb.subs = subs
                        grp.append(jb)
                    assign_flags(grp)
                    grp[-1].epi = make_epi(hh, j, comp, len(grp) < 20)
                    jobs.extend(grp)
            run_attn(K, jobs, PT, b_PT, [0, 1, 2])
        if state["deferred"] is not None:
            state["deferred"]()
        P.barrier()
        P.flush(K.block)


def phase_nsa(K):
    nc, P, I, Sc, ps, pb = K.nc, K.P, K.I, K.Sc, K.ps, K.pb
    with contextlib.ExitStack() as ph:
        sb = lambda n, s, d=F32, stk=ph: K.sb(n, s, d, stk)
        KS = sb("KS", [128, S], BF16); KW = sb("KW", [128, S], BF16); b_KS = P.buf(); b_KW = P.buf()
        VS = sb("VS", [128, 64, 65], BF16); VW = sb("VW", [128, 64, 65], BF16); b_VS = P.buf(); b_VW = P.buf()
        Qh = [sb(f"Qh{i}", [128, NQ], BF16) for i in range(4)]; b_Qh = P.bufs(4)
        KC = sb("KC", [128, 512], BF16); b_KC = P.buf()
        VC = sb("VC", [128, 4, 193], BF16); b_VC = P.buf()
        dm = sb("dm_n", [128, 8, 512], BF16); wm = sb("wm_n", [128, 12, 512], BF16)
        QW = [[sb(f"QW{w_}{h_}", [128, 512], BF16) for h_ in range(4)] for w_ in range(4)]
        b_QW = [[P.buf() for h_ in range(4)] for w_ in range(4)]
        cm = sb("cm_n", [128, 3, 512], BF16); addm = sb("addm", [128, 32, 128], BF16); b_const = P.buf()
        cposT = sb("cposT_s", [128, 4])
        PT = [sb(f"PTn{i}", [128, 512], BF16) for i in range(6)]; b_PT = P.bufs(6)
        bKs = sb("bKs", [128, 4, 64]); bC = sb("bC", [128, 4, 4]); b_bias = P.buf()
        ocomb = [sb(f"ocomb{i}", [128, 4, 4, 64]) for i in range(2)]; b_oc = P.bufs(2)
        imp = sb("imp", [128, 4, 128]); b_imp = P.buf()
        scr = sb("scr", [128, 128]); scr2 = sb("scr2", [128, 128]); t8 = sb("t8", [128, 16]); b_sel = P.buf()
        selb = sb("selb", [128, 4, 128]); b_selb = P.buf()
        rzt = sb("rzt", [128, 8]); b_rzt = P.buf()
        evn = [sb(f"evn{i}", [128, 4, 193]) for i in range(3)]; b_evn = P.bufs(3)
        est = {"e": 0}
        obst = [sb(f"obst{i}", [128, 512], BF16) for i in range(2)]; b_obst = P.bufs(2)
        P.dma(dm[:], I["dmask"], W=[b_const]); P.dma(wm[:], I["wmask"], W=[b_const])
        P.dma(cm[:], I["cmask"], W=[b_const]); P.dma(addm[:], I["addmask"], W=[b_const]); P.dma(cposT[:], I["cposT"], W=[b_const])
        P.memset(KS[64:96, :], 0.0, W=[b_KS]); P.memset(KS[64:65, :], 1.0, W=[b_KS]); P.memset(KW[64:65, :], 1.0, W=[b_KW])
        P.dma(KS[96:128, :], I["wind"], W=[b_KS])
        for w_ in range(4):
            for h_ in range(4):
                P.memset(QW[w_][h_][64:96, :], 0.0, W=[b_QW[w_][h_]])
        P.memset(VS[:, :, 64:65], 1.0, W=[b_VS], eng="dve"); P.memset(VW[:, :, 64:65], 1.0, W=[b_VW], eng="dve")
        P.memset(VC[:, :, 192:193], 1.0, W=[b_VC], eng="dve")
        P.dma(VC[:, :, 64:192], I["c2s"], W=[b_VC])
        ntm_v = Sc["ntm"].rearrange("(n r) d -> r n d", r=128)
        accb = [3, 4, 5, 6]
        TB = 7
        rot = Rot([3, 4, 5, 6, 7])

        ctmp = {}
        cbufs = P.bufs(5)
        for g in range(2):
            P.dma(VS[:, :, 0:64], ntm_v[:, :, g * 64:(g + 1) * 64], W=[b_VS])
            P.dma(VW[:, :, 0:64], ntm_v[:, :, 128 + g * 64:128 + (g + 1) * 64], W=[b_VW])
            for hg in range(4):
                hh = g * 4 + hg
                m = float(SL_B[hh])
                P.dma(Qh[hg][0:64, :], Sc["qtn"][hh // 2, (hh % 2) * 64:(hh % 2 + 1) * 64, :], W=[b_Qh[hg]])
                P.dma(Qh[hg][64:65, :], I["refn"][hh:hh + 1, :], W=[b_Qh[hg]])
                P.ts(bKs[:, hg, :], K.kposT[:], m, None, ALU.mult, R=[K.b_pos], W=[b_bias], eng="pool")
                P.ts(bC[:, hg, :], cposT[:], m, None, ALU.mult, R=[b_const], W=[b_bias], eng="pool")
            P.memset(KC[:], 0.0, W=[b_KC]); P.memset(KC[64:65, :], 1.0, W=[b_KC])
            for kind in range(2):
                if True:
                    def sbc(n, s, d=F32):
                        if n not in ctmp:
                            ctmp[n] = K.sb("c_" + n, s, d, ph)
                        return ctmp[n]
                    cT = KS if kind == 0 else KW; w1 = sbc("w1", [128, 32, 128], BF16); w2 = sbc("w2", [128, 64], BF16)
                    peT = sbc("peT", [128, 32], BF16); hf = sbc("hf", [128, 512]); t1 = sbc("t1", [128, 512])
                    hid = sbc("hid", [128, 512], BF16); cst = sbc("cst", [128, 1])
                    b_c = b_KS if kind == 0 else b_KW
                    b_w, b_h, b_t, b_hid, b_cst = cbufs
                    pre = "ck" if kind == 0 else "cv"
                    P.dma(cT[0:64, :], Sc["nfm"][kind, g * 64:(g + 1) * 64, :], W=[b_c])
                    P.dma(w1[0:64, :, :], I[pre + "_w1"].rearrange("(l d) h -> d l h", d=64), W=[b_w], eng="pool")
                    P.dma(w2[:], I[pre + "_w2"], W=[b_w], eng="pool")
                    P.dma(peT[0:64, :], I[pre + "_peT"], W=[b_w], eng="pool")
                    bank = rot.next()
                    for l in range(32):
                        P.mm(ps[:, bank, 0:1], w1[0:64, l, :], peT[0:64, l:l + 1], l == 0, l == 31, R=[b_w], W=[pb[bank]])
                    P.copy(cst[:], ps[:, bank, 0:1], R=[pb[bank]], W=[b_cst])
                    bank = rot.next()
                    cTr = cT[0:64, :].rearrange("p (c s) -> p c s", s=16)
                    for l in range(32):
                        rhs = cTr[:, 0:511, l] if l < 16 else cTr[:, 1:512, l - 16]
                        P.mm(ps[:, bank, 0:511], w1[0:64, l, :], rhs, l == 0, l == 31, R=[b_w, b_c], W=[pb[bank]])
                    P.memset(hf[:], 0.0, W=[b_h])
                    P.act(hf[:, 0:511], ps[:, bank, 0:511], AF.Identity, R=[pb[bank], b_cst], W=[b_h], bias=cst[:, 0:1], scale=1.0)
                    P.tt(t1[:], hf[:], hf[:], ALU.mult, R=[b_h], W=[b_t])
                    P.ts(t1[:], t1[:], 0.044715, 1.0, ALU.mult, ALU.add, R=[b_t], W=[b_t])
                    P.tt(t1[:], t1[:], hf[:], ALU.mult, R=[b_t, b_h], W=[b_t])
                    P.act(t1[:], t1[:], AF.Sigmoid, R=[b_t], W=[b_t], scale=1.5957691216057308)
                    P.tt(hid[:], hf[:], t1[:], ALU.mult, R=[b_t, b_h], W=[b_hid])
                    if kind == 0:
                        bank = rot.next()
                        P.mm(ps[0:64, bank, 0:511], w2[:, 0:64], hid[:, 0:511], True, True, R=[b_w, b_hid], W=[pb[bank]])
                        P.copy(KC[0:64, 0:511], ps[0:64, bank, 0:511], R=[pb[bank]], W=[b_KC])
                    else:
                        bank = rot.next()
                        for tau in range(4):
                            P.mm(ps[:, bank, tau * 64:(tau + 1) * 64], hid[:, tau * 128:(tau + 1) * 128], w2[:, :], True, True, R=[b_w, b_hid], W=[pb[bank]])
                        P.copy(VC[:, :, 0:64], ps[:, bank, 0:256].rearrange("p (t d) -> p t d", d=64), R=[pb[bank]], W=[b_VC])
            P.dma(KS[0:64, :], Sc["nfm"][2, g * 64:(g + 1) * 64, :], W=[b_KS])
            P.dma(KW[0:64, :], Sc["nfm"][3, g * 64:(g + 1) * 64, :], W=[b_KW])
            if K.debug:
                P.dma(K.dbg_kc[g], KC[0:64, :], R=[b_KC]); P.dma(K.dbg_vc[g], VC[:, :, 0:64], R=[b_VC])
            state = {"deferred": None}
            jobs = []
            for j in range(8):
                k = j % 2
                qs = slice(j * 512, (j + 1) * 512)

                def cmp_epi(hg, j=j, k=k):
                    def epi():
                        hh = g * 4 + hg
                        x = est["e"] % 3
                        est["e"] += 1
                        for sub in range(4):
                            P.act(evn[x][:, sub, :], ps[:, accb[sub], 0:193], AF.Copy, R=[pb[accb[sub]]], W=[b_evn[x]])
                        for sub in range(4):
                            tile = j * 4 + sub
                            P.ts(rzt[:, sub:sub + 1], evn[x][:, sub, 192:193], 1e-30, None, ALU.max, R=[b_evn[x]], W=[b_rzt])
                            P.op("dve", lambda e, sub=sub: e.reciprocal(out=rzt[:, sub:sub + 1], in_=rzt[:, sub:sub + 1]), [b_rzt], [b_rzt])
                            if hg == 0:
                                P.ts(imp[:, sub, :], evn[x][:, sub, 64:192], rzt[:, sub:sub + 1], None, ALU.mult, R=[b_evn[x], b_rzt], W=[b_imp])
                            else:
                                P.stt(imp[:, sub, :], evn[x][:, sub, 64:192], rzt[:, sub:sub + 1], imp[:, sub, :], ALU.mult, ALU.add, R=[b_evn[x], b_rzt, b_imp], W=[b_imp])
                            P.ts(ocomb[k][:, sub, hg, :], evn[x][:, sub, 0:64], rzt[:, sub:sub + 1], K.gates[:, tile, hh * 3:hh * 3 + 1], ALU.mult, ALU.mult, R=[b_evn[x], b_rzt, K.b_gates], W=[b_oc[k]])
                        if hg == 3:
                            for sub in range(4):
                                tile = j * 4 + sub
                                P.tt(scr[:], imp[:, sub, :], addm[:, tile, :], ALU.add, R=[b_imp, b_const], W=[b_sel])
                                P.op("dve", lambda e: e.max(out=t8[:, 0:8], in_=scr[:]), [b_sel], [b_sel])
                                P.op("dve", lambda e: e.match_replace(out=scr2[:], in_to_replace=t8[:, 0:8], in_values=scr[:], imm_value=-3.0e38), [b_sel], [b_sel])
                                P.op("dve", lambda e: e.max(out=t8[:, 8:16], in_=scr2[:]), [b_sel], [b_sel])
                                P.ts(selb[:, sub, :], scr[:], t8[:, 15:16], NEG, ALU.is_lt, ALU.mult, R=[b_sel], W=[b_selb])
                    return epi

                def br_epi(hg, br, short, j=j, k=k):
                    def epi():
                        hh = g * 4 + hg
                        x = est["e"] % 3
                        est["e"] += 1
                        for sub in range(4):
                            if short:
                                P.act(evn[x][:, sub, 0:65], ps[:, accb[sub], 0:65], AF.Copy, R=[pb[accb[sub]]], W=[b_evn[x]])
                            else:
                                P.copy(evn[x][:, sub, 0:65], ps[:, accb[sub], 0:65], R=[pb[accb[sub]]], W=[b_evn[x]])
                        for sub in range(4):
                            tile = j * 4 + sub
                            c = 4 + sub
                            P.op("dve", lambda e, sub=sub, c=c: e.reciprocal(out=rzt[:, c:c + 1], in_=evn[x][:, sub, 64:65]), [b_evn[x]], [b_rzt])
                            P.tt(rzt[:, c:c + 1], rzt[:, c:c + 1], K.gates[:, tile, hh * 3 + br:hh * 3 + br + 1], ALU.mult, R=[b_rzt, K.b_gates], W=[b_rzt])
                            P.stt(ocomb[k][:, sub, hg, :], evn[x][:, sub, 0:64], rzt[:, c:c + 1], ocomb[k][:, sub, hg, :], ALU.mult, ALU.add, R=[b_evn[x], b_rzt, b_oc[k]], W=[b_oc[k]])
                        if br == 1 and hg == 3:
                            def deferred(j=j, k=k):
                                for pp in range(2):
                                    for sub in range(4):
                                        P.tr(ps[:, TB, sub * 128:(sub + 1) * 128], ocomb[k][:, sub, 2 * pp:2 * pp + 2, :].rearrange("p a b -> p (a b)"), K.ident[:], R=[b_oc[k], K.b_ident], W=[pb[TB]])
                                    o = (j * 2 + pp) % 2
                                    P.copy(obst[o][:], ps[:, TB, :], R=[pb[TB]], W=[b_obst[o]])
                                    P.dma(Sc["obt"][g * 2 + pp, :, j * 512:(j + 1) * 512], obst[o][:], R=[b_obst[o]])
                            prev = state["deferred"]
                            state["deferred"] = deferred
                            if prev is not None:
                                prev()
                    return epi

                def sel_pre(j=j, k=k):
                    def pre():
                        for sub in range(4):
                            P.tr(ps[:, TB, sub * 128:(sub + 1) * 128], selb[:, sub, :], K.ident[:], R=[b_selb, K.b_ident], W=[pb[TB]])
                        for hg in range(4):
                            tf = first_tile(j, float(SL_B[g * 4 + hg]))
                            for w_ in range(tf // 16, (8 * j + 7) // 16 + 1):
                                P.copy(QW[w_][hg][0:65, :], Qh[hg][0:65, j * 512:(j + 1) * 512], R=[b_Qh[hg]], W=[b_QW[w_][hg]], eng="pool")
                                P.copy(QW[w_][hg][96:128, :], ps[32 * w_:32 * w_ + 32, TB, :], R=[pb[TB]], W=[b_QW[w_][hg]])
                    return pre

                tmax = (64 * j + 62) // 128
                for hg in range(4):
                    for tau in range(tmax + 1):
                        masks = []
                        if j % 2 == 0:
                            if tau == j // 2:
                                masks = [(K.identb[:], cm[:, 1, :], [K.b_ident, b_const])]
                            elif tau == j // 2 - 1:
                                masks = [(K.identb[:], cm[:, 0, :], [K.b_ident, b_const])]
                        else:
                            if tau == (j - 1) // 2:
                                masks = [(K.identb[:], cm[:, 2, :], [K.b_ident, b_const])]
                        jobs.append(mkjob(KC[0:65, tau * 128:(tau + 1) * 128], Qh[hg][0:65, qs], masks, bC[:, hg, tau:tau + 1], VC[:, tau, :], 193, accb,
                                          tau == 0, tau == tmax, [b_KC, b_Qh[hg]], [b_bias], [b_VC], cmp_epi(hg) if tau == tmax else None))
                for hg in range(4):
                    t0 = max(0, 8 * j - 4, first_tile(j, float(SL_B[g * 4 + hg])))
                    grp = []
                    for t in range(max(0, 8 * j - 4), 8 * j + 8):
                        subs = band_subs(t - 8 * j, float(SL_B[g * 4 + hg]), window=True)
                        if not subs:
                            continue
                        masks = [(K.identb[:], wm[:, t - (8 * j - 4), :], [K.b_ident, b_const])]
                        jb = mkjob(KW[0:65, t * 128:(t + 1) * 128], Qh[hg][0:65, qs], masks, bKs[:, hg, t:t + 1], VW[:, t, :], 65, accb,
                                   t == t0, t == 8 * j + 7, [b_KW, b_Qh[hg]], [b_bias], [b_VW], None)
                        jb.subs = subs
                        grp.append(jb)
                    assign_flags(grp)
                    grp[-1].epi = br_epi(hg, 2, len(grp) < 20)
                    jobs.extend(grp)
                for hg in range(4):
                    tf = first_tile(j, float(SL_B[g * 4 + hg]))
                    sgrp = []
                    for t in range(0, 8 * j + 8):
                        subs = band_subs(t - 8 * j, float(SL_B[g * 4 + hg]))
                        if not subs:
                            continue
                        masks = []
                        if t >= 8 * j:
                            masks.append((K.identb[:], dm[:, t - 8 * j, :], [K.b_ident, b_const]))
                        jb = mkjob(KS[:, t * 128:(t + 1) * 128], QW[t // 16][hg][:, :], masks, bKs[:, hg, t:t + 1], VS[:, t, :], 65, accb,
                                   t == tf, t == 8 * j + 7, [b_KS, b_QW[t // 16][hg]], [b_bias], [b_VS], None)
                        if hg == 0 and not sgrp:
                            jb.pre = sel_pre()
                        jb.subs = subs
                        sgrp.append(jb)
                    assign_flags(sgrp)
                    sgrp[-1].epi = br_epi(hg, 1, len(sgrp) < 20)
                    jobs.extend(sgrp)
            run_attn(K, jobs, PT, b_PT, [0, 1, 2])
            if state["deferred"] is not None:
                state["deferred"]()
            P.barrier()
            P.flush(K.block)


def phase_d(K):
    nc, P, I, Sc, ps, pb = K.nc, K.P, K.I, K.Sc, K.ps, K.pb
    with contextlib.ExitStack() as ph:
        sb = lambda n, s, d=F32: K.sb(n, s, d, ph)
        wda = sb("wda", [128, 8, D], BF16); wnsa = sb("wnsa", [128, 4, D], BF16); wo = sb("wo", [128, 8, D], BF16); b_w = P.buf()
        rw = sb("rw", [128, 8, 64]); rwh = sb("rwh", [128, 8, 64], BF16); rwl = sb("rwl", [128, 8, 64], BF16); b_rw = P.buf()
        for (dst, src, n) in [(wda, I["w_da_out"], 8), (wnsa, I["w_nsa_out"], 4), (wo, I["w_o"], 8)]:
            v = src.rearrange("(h p) f -> p h f", p=128)
            for half in range(2):
                P.dma(dst[:, :, half * 512:(half + 1) * 512], v[:, :, half * 512:(half + 1) * 512], W=[b_w], eng="pool")
        for kc in range(8):
            P.tt(wo[:, kc, :], wo[:, kc, :], K.g1bc[:], ALU.mult, R=[K.b_gbc, b_w], W=[b_w])
        P.dma(rw[:], I["router_w"].rearrange("(kc p) e -> p kc e", p=128), W=[b_rw])
        P.copy(rwh[:], rw[:], R=[b_rw], W=[b_rw])
        P.tt(rwl[:], rw[:], rwh[:], ALU.subtract, R=[b_rw], W=[b_rw])
        oaT = [sb(f"oaT{i}", [128, 8, 512], BF16) for i in range(2)]; obT = [sb(f"obT{i}", [128, 4, 512], BF16) for i in range(2)]
        mgt = [sb(f"mgt{i}", [128, 2, 512]) for i in range(5)]; b_mgt = P.bufs(5); xo = [sb(f"xo{i}", [128, 4, D]) for i in range(2)]
        b_in = P.bufs(2); b_xo = P.bufs(2)
        mT = sb("mT", [128, 8, 512], BF16); b_mT = P.buf()
        tA = [sb(f"tA{i}", [128, 512]) for i in range(2)]; tB = [sb(f"tB{i}", [128, 512]) for i in range(2)]; b_tA = P.bufs(2); b_tB = P.bufs(2)
        hT = sb("h2T", [128, 8, 512], BF16); b_hT = P.buf()
        hf = [sb(f"h2f{i}", [128, 512]) for i in range(2)]; b_hf = P.bufs(2); hl = sb("h2l", [128, 8, 512], BF16); b_hl = P.buf()
        ss = sb("ssd", [128, 4]); sd = sb("sdd", [128, 4]); rs = sb("rsd", [128, 4]); b_st = P.buf()
        junk = sb("junkd", [128, D], BF16); b_junk = P.buf()
        r1 = sb("r1", [128, 64]); r2 = sb("r2", [128, 64]); r3 = sb("r3", [128, 64]); r4 = sb("r4", [128, 64]); rg = sb("rg", [128, 40]); b_r = P.buf()
        oat_v = Sc["oat"].rearrange("h p t -> p h t"); obt_v = Sc["obt"].rearrange("h p t -> p h t")
        xo_v = I["xo"].rearrange("(c s r) d -> c r s d", s=4, r=128)
        x1_v = Sc["x1"].rearrange("(c s r) d -> c r s d", s=4, r=128); h2t_v = Sc["h2t"].rearrange("k p t -> p k t")
        rot = Rot(range(8))

        def load(j):
            s = j % 2
            qs = slice(j * 512, (j + 1) * 512)
            P.dma(oaT[s][:], oat_v[:, :, qs], W=[b_in[s]]); P.dma(obT[s][:], obt_v[:, :, qs], W=[b_in[s]])
            P.dma(xo[s][:], xo_v[j], W=[b_xo[s]])
        def stage1(j):
            s = j % 2
            for fc in range(8):
                mi = (j * 8 + fc) % 5
                P.dma(mgt[mi][:, 0, :], Sc["mg"][fc, :, j * 512:(j + 1) * 512], W=[b_mgt[mi]])
                P.dma(mgt[mi][:, 1, :], Sc["mg"][8 + fc, :, j * 512:(j + 1) * 512], W=[b_mgt[mi]])
                bA = rot.next()
                for hh in range(8):
                    P.mm(ps[:, bA, :], wda[:, hh, fc * 128:(fc + 1) * 128], oaT[s][:, hh, :], hh == 0, hh == 7, R=[b_w, b_in[s]], W=[pb[bA]])
                bB = rot.next()
                for pr in range(4):
                    P.mm(ps[:, bB, :], wnsa[:, pr, fc * 128:(fc + 1) * 128], obT[s][:, pr, :], pr == 0, pr == 3, R=[b_w, b_in[s]], W=[pb[bB]])
                q = fc % 2
                P.tt(tA[q][:], ps[:, bA, :], mgt[mi][:, 0, :], ALU.mult, R=[pb[bA], b_mgt[mi]], W=[b_tA[q]])
                P.tt(tB[q][:], ps[:, bB, :], mgt[mi][:, 1, :], ALU.mult, R=[pb[bB], b_mgt[mi]], W=[b_tB[q]])
                P.tt(mT[:, fc, :], tA[q][:], tB[q][:], ALU.add, R=[b_tA[q], b_tB[q]], W=[b_mT], eng="pool")
            for sub in range(4):
                for half in range(2):
                    bank = rot.next()
                    for fc in range(8):
                        P.mm(ps[:, bank, :], mT[:, fc, sub * 128:(sub + 1) * 128], wo[:, fc, half * 512:(half + 1) * 512], fc == 0, fc == 7, R=[b_mT, b_w], W=[pb[bank]])
                    P.tt(xo[s][:, sub, half * 512:(half + 1) * 512], ps[:, bank, :], xo[s][:, sub, half * 512:(half + 1) * 512], ALU.add, R=[pb[bank], b_xo[s]], W=[b_xo[s]])
            P.dma(x1_v[j], xo[s][:], R=[b_xo[s]])

        def stage2(j):
            s = j % 2
            norm_chunk(K, xo[s], b_xo[s], hT, b_hT, ss, sd, rs, b_st, junk, b_junk, K.a2, K.modT[:, 24:32], rot, hT_lo=(hf, b_hf, hl, b_hl))
            P.dma(h2t_v[:, :, j * 512:(j + 1) * 512], hT[:], R=[b_hT])
            for sub in range(4):
                tile = j * 4 + sub
                bank = rot.next()
                cs = slice(sub * 128, (sub + 1) * 128)
                n = 0
                for kc in range(8):
                    for (l, r, bl) in [(hT, rwh, b_hT), (hT, rwl, b_hT), (hl, rwh, b_hl)]:
                        P.mm(ps[:, bank, 0:64], l[:, kc, cs], r[:, kc, :], n == 0, n == 23, R=[bl, b_rw], W=[pb[bank]])
                        n += 1
                route(K, ps[:, bank, 0:64], pb[bank], tile, r1, r2, r3, r4, rg, b_r)

        load(0)
        load(1)
        stage1(0)
        for j in range(8):
            if j + 1 < 8:
                stage1(j + 1)
            stage2(j)
            if j + 2 < 8:
                load(j + 2)
        P.barrier()
        P.flush(K.block)


def route(K, logits, b_log, tile, r1, r2, r3, r4, rg, b_r):
    P = K.P
    R = [b_r]
    P.act(r1[:], logits, AF.Sigmoid, R=[b_log], W=[b_r])
    P.tt(r2[:], r1[:], K.rbbc[:], ALU.add, R=[b_r, K.b_rb], W=[b_r])
    r2g = r2[:].rearrange("p (g e) -> p g e", e=8)
    r3g = r3[:].rearrange("p (g e) -> p g e", e=8)
    P.op("dve", lambda e: e.tensor_reduce(out=rg[:, 0:8], in_=r2g, axis=AX.X, op=ALU.max), R, R)
    P.tt(r3g, r2g, rg[:, 0:8].unsqueeze(2).to_broadcast([128, 8, 8]), ALU.is_equal, R=R, W=R)
    P.stt(r3[:], r3[:], -1.0e9, r2[:], ALU.mult, ALU.add, R=R, W=R)
    P.op("dve", lambda e: e.tensor_reduce(out=rg[:, 8:16], in_=r3g, axis=AX.X, op=ALU.max), R, R)
    P.tt(rg[:, 16:24], rg[:, 0:8], rg[:, 8:16], ALU.add, R=R, W=R)
    P.op("dve", lambda e: e.max(out=rg[:, 24:32], in_=rg[:, 16:24]), R, R)
    P.ts(rg[:, 32:40], rg[:, 16:24], rg[:, 27:28], -1.0e30, ALU.is_lt, ALU.mult, R=R, W=R)
    P.tt(r3g, r2g, rg[:, 32:40].unsqueeze(2).to_broadcast([128, 8, 8]), ALU.add, R=R, W=R)
    P.op("dve", lambda e: e.max(out=rg[:, 0:8], in_=r3[:]), R, R)
    P.ts(r4[:], r3[:], rg[:, 7:8], None, ALU.is_ge, R=R, W=R)
    P.tt(r4[:], r4[:], r1[:], ALU.mult, R=R, W=R)
    P.op("dve", lambda e: e.tensor_reduce(out=rg[:, 8:9], in_=r4[:], axis=AX.X, op=ALU.add), R, R)
    P.op("dve", lambda e: e.reciprocal(out=rg[:, 9:10], in_=rg[:, 8:9]), R, R)
    P.ts(K.Wr[:, tile, :], r4[:], rg[:, 9:10], 2.5, ALU.mult, ALU.mult, R=R, W=[K.b_Wr])


def phase_moe(K, out_d):
    nc, P, I, Sc, ps, pb = K.nc, K.P, K.I, K.Sc, K.ps, K.pb
    with contextlib.ExitStack() as ph:
        sb = lambda n, s, d=F32: K.sb(n, s, d, ph)
        h2 = sb("h2m", [128, 8, 2048], BF16); b_h2 = P.buf()
        acc = sb("accm", [128, 16, D]); b_acc = P.bufs(16)
        NW = 3
        wg = [sb(f"wg{i}", [128, 8, 256], BF16) for i in range(NW)]; wu = [sb(f"wu{i}", [128, 8, 256], BF16) for i in range(NW)]
        wd = [sb(f"wd{i}", [128, 2, D], BF16) for i in range(NW)]; b_wt = P.bufs(NW)
        sg = [sb(f"sg{i}", [128, 512]) for i in range(2)]; b_sg = P.bufs(2)
        aT = [sb(f"aT{i}", [128, 512], BF16) for i in range(2)]; b_aT = P.bufs(2)
        x1t = [sb(f"x1t{i}", [128, D]) for i in range(2)]; b_x1 = P.bufs(2)
        ss = sb("ssm", [128, 2]); b_ss = P.buf()
        junk = sb("junkm", [128, D], BF16); b_junk = P.buf()
        h2t_v = Sc["h2t"].rearrange("k p t -> p k t")
        x1_v = Sc["x1"].rearrange("(t r) d -> t r d", r=128)
        out_v = out_d.rearrange("(t r) d -> t r d", r=128)
        grot = Rot([0, 1]); urot = Rot([2, 3]); drot = Rot([4, 5, 6, 7])
        order = [64] + list(range(64))
        outs = []

        def wload(idx):
            e = order[idx % 65]
            s = idx % NW
            if e == 64:
                g_ap, u_ap, d_ap = I["sg"], I["su"], I["sd"]
            else:
                g_ap, u_ap, d_ap = I["eg"][e], I["eu"][e], I["ed"][e]
            P.dma(wg[s][:], g_ap.rearrange("(kc p) h -> p kc h", p=128), W=[b_wt[s]], eng="pool")
            P.dma(wu[s][:], u_ap.rearrange("(kc p) h -> p kc h", p=128), W=[b_wt[s]], eng="pool")
            dv = d_ap.rearrange("(hc p) f -> p hc f", p=128)
            P.dma(wd[s][:, :, 0:512], dv[:, :, 0:512], W=[b_wt[s]], eng="pool")
            P.dma(wd[s][:, :, 512:1024], dv[:, :, 512:1024], W=[b_wt[s]], eng="pool")

        total = 2 * 65
        wload(0); wload(1)

        def gu(sc, ei, ch, s):
            bG = grot.next(); bU = urot.next()
            ts_ = slice(ch * 256, (ch + 1) * 256)
            for hc in range(2):
                for kc in range(8):
                    P.mm(ps[:, bG, hc * 256:(hc + 1) * 256], wg[s][:, kc, hc * 128:(hc + 1) * 128], h2[:, kc, ts_], kc == 0, kc == 7, R=[b_wt[s], b_h2], W=[pb[bG]])
            for hc in range(2):
                for kc in range(8):
                    P.mm(ps[:, bU, hc * 256:(hc + 1) * 256], wu[s][:, kc, hc * 128:(hc + 1) * 128], h2[:, kc, ts_], kc == 0, kc == 7, R=[b_wt[s], b_h2], W=[pb[bU]])
            return bG, bU

        def rest(sc, ei, ch, s, bG, bU, q):
            e = order[ei]
            P.act(sg[q][:], ps[:, bG, :], AF.Silu, R=[pb[bG]], W=[b_sg[q]])
            P.tt(aT[q][:], sg[q][:], ps[:, bU, :], ALU.mult, R=[b_sg[q], pb[bU]], W=[b_aT[q]])
            for sub in range(2):
                lt = ch * 2 + sub
                gt = sc * 16 + lt
                for half in range(2):
                    bank = drot.next()
                    for hc in range(2):
                        P.mm(ps[:, bank, :], aT[q][:, hc * 256 + sub * 128:hc * 256 + (sub + 1) * 128], wd[s][:, hc, half * 512:(half + 1) * 512], hc == 0, hc == 1, R=[b_aT[q], b_wt[s]], W=[pb[bank]])
                    dst = acc[:, lt, half * 512:(half + 1) * 512]
                    if ei == 0:
                        P.copy(dst, ps[:, bank, :], R=[pb[bank]], W=[b_acc[lt]])
                    else:
                        P.stt(dst, ps[:, bank, :], K.Wr[:, gt, e:e + 1], dst, ALU.mult, ALU.add, R=[pb[bank], K.b_Wr, b_acc[lt]], W=[b_acc[lt]])

        def epilogue_tile(sc, lt):
            gt = sc * 16 + lt
            q = lt % 2
            P.dma(x1t[q][:], x1_v[gt], W=[b_x1[q]])
            P.tt(acc[:, lt, :], acc[:, lt, :], K.g2bc[:], ALU.mult, R=[b_acc[lt], K.b_gbc], W=[b_acc[lt]])
            P.tt(acc[:, lt, :], acc[:, lt, :], x1t[q][:], ALU.add, R=[b_acc[lt], b_x1[q]], W=[b_acc[lt]])
            P.act(junk[:], acc[:, lt, :], AF.Square, R=[b_acc[lt]], W=[b_junk, b_ss], accum_out=ss[:, 0:1])
            P.act(ss[:, 1:2], ss[:, 0:1], AF.Sqrt, R=[b_ss, K.b_eps], W=[b_ss], scale=1.0 / D, bias=K.epsc[:, 0:1])
            P.op("dve", lambda e: e.reciprocal(out=ss[:, 1:2], in_=ss[:, 1:2]), [b_ss], [b_ss])
            P.stt(acc[:, lt, :], acc[:, lt, :], ss[:, 1:2], K.fnwbc[:], ALU.mult, ALU.mult, R=[b_acc[lt], b_ss, K.b_gbc], W=[b_acc[lt]])
            outs.append(P.dma(out_v[gt], acc[:, lt, :], R=[b_acc[lt]]))

        def rest_and_epilogue(sc, ei, ch, s_, bG, bU, q):
            rest(sc, ei, ch, s_, bG, bU, q)
            if ei == 64:
                epilogue_tile(sc, ch * 2)
                epilogue_tile(sc, ch * 2 + 1)

        idx = 0
        it = 0
        for sc in range(2):
            P.dma(h2[:, 0:4, :], h2t_v[:, 0:4, sc * 2048:(sc + 1) * 2048], W=[b_h2])
            P.dma(h2[:, 4:8, :], h2t_v[:, 4:8, sc * 2048:(sc + 1) * 2048], W=[b_h2])
            steps = []
            for ei in range(65):
                for ch in range(8):
                    steps.append((ei, ch))
            pend = None
            for si, (ei, ch) in enumerate(steps):
                if ch == 0:
                    cur_s = idx % NW
                    idx += 1
                if ch == 1 and idx + 1 < total:
                    wload(idx + 1)
                bG, bU = gu(sc, ei, ch, cur_s)
                if pend is not None:
                    rest_and_epilogue(*pend)
                pend = (sc, ei, ch, cur_s, bG, bU, it % 2)
                it += 1
            rest_and_epilogue(*pend)
        P.wait_all("sp", outs)
        P.barrier()
        P.flush(K.block)


def _own_rows(p):
    r = np.arange(128)
    return np.concatenate([(2 * i + p) * 128 + r for i in range(32)])


def _consts(p):
    bf = ml_dtypes.bfloat16
    r = np.arange(128)
    qpos = _own_rows(p).astype(np.float64)
    c = {}
    c["refda"] = np.stack([-SL_A[h] * qpos for h in range(8)]).astype(np.float32).astype(bf)
    c["refn"] = np.stack([-SL_B[h] * qpos for h in range(8)]).astype(np.float32).astype(bf)
    c["kposT"] = (np.arange(64)[None, :] * 128 + r[:, None]).astype(np.float32)
    cc = np.arange(512).reshape(4, 128).T
    c["cposT"] = np.where(cc < 511, 16.0 * cc + 31.0, -1.0e6).astype(np.float32)
    qrel = np.concatenate([128 * (2 * sub + p) + r for sub in range(4)])
    dm = np.zeros((128, 8, 512), np.float32)
    for kt in range(8):
        krel = 128 * kt + r
        dm[:, kt, :] = np.where(krel[:, None] <= qrel[None, :], 0.0, NEG)
    c["dmask"] = dm.astype(bf)
    wm = np.zeros((128, 12, 512), np.float32)
    for kw in range(12):
        krel = 128 * (kw - 4) + r
        d = qrel[None, :] - krel[:, None]
        wm[:, kw, :] = np.where((d >= 0) & (d < 512), 0.0, NEG)
    c["wmask"] = wm.astype(bf)
    cmk = np.zeros((128, 3, 512), np.float32)
    for i, off in enumerate([128, 0, 64]):
        relc = 16 * (r - off) + 31
        cmk[:, i, :] = np.where(qrel[None, :] >= relc[:, None], 0.0, NEG)
    c["cmask"] = cmk.astype(bf)
    t = qpos.reshape(32, 128).T.astype(np.int64)
    cur = t // 64
    n = np.arange(128)[None, None, :]
    forced = (n == 0) | (n == cur[:, :, None]) | (n == cur[:, :, None] - 1)
    causal = n <= cur[:, :, None]
    c["addmask"] = np.where(causal, 1.0e4 * forced, -1.0e30).astype(np.float32).astype(bf)
    k = np.arange(S)
    c["wind"] = (((k[None, :] // 64) % 32) == np.arange(32)[:, None]).astype(np.float32).astype(bf)
    ci = np.arange(512)
    c0 = ci[:, None] * 16
    s0 = np.arange(128)[None, :] * 64
    ov = np.minimum(c0 + 32, s0 + 64) - np.maximum(c0, s0)
    M = np.clip(ov, 0, None).astype(np.float32) / 32.0
    M[511, :] = 0.0
    c["c2s"] = np.ascontiguousarray(M.reshape(4, 128, 128).transpose(1, 0, 2)).astype(bf)
    return c


def make_in_maps(inputs):
    f = lambda a: np.ascontiguousarray(np.asarray(a, dtype=np.float32))
    x = f(inputs["x"]); cvec = f(inputs["c"])
    shared = {
        "ada_w": f(inputs["ada_w"][0]), "ada_bT": f(inputs["ada_b"][0].reshape(48, 128).T), "ada_b": f(inputs["ada_b"][0].reshape(1, -1)),
        "n1wT": f(inputs["norm1_w"][0].reshape(8, 128).T), "n2wT": f(inputs["norm2_w"][0].reshape(8, 128).T),
        "w_in": f(inputs["w_in"][0]),
        "lq1": f(inputs["da_lq1"][0].reshape(1, 64)), "lk1": f(inputs["da_lk1"][0].reshape(1, 64)),
        "lq2": f(inputs["da_lq2"][0].reshape(1, 64)), "lk2": f(inputs["da_lk2"][0].reshape(1, 64)),
        "subln": f(inputs["da_subln_w"][0].reshape(1, 128)),
        "ck_peT": f(inputs["cmp_k_pe"][0].T), "ck_w1": f(inputs["cmp_k_w1"][0]), "ck_w2": f(inputs["cmp_k_w2"][0]),
        "cv_peT": f(inputs["cmp_v_pe"][0].T), "cv_w1": f(inputs["cmp_v_w1"][0]), "cv_w2": f(inputs["cmp_v_w2"][0]),
        "w_da_out": f(inputs["w_da_out"][0]), "w_nsa_out": f(inputs["w_nsa_out"][0]), "w_o": f(inputs["w_o"][0]),
        "router_w": f(inputs["router_w"][0]), "router_b": f(inputs["router_b"][0].reshape(1, 64)),
        "eg": f(inputs["exp_w_gate"][0]), "eu": f(inputs["exp_w_up"][0]), "ed": f(inputs["exp_w_down"][0]),
        "sg": f(inputs["sh_w_gate"][0]), "su": f(inputs["sh_w_up"][0]), "sd": f(inputs["sh_w_down"][0]),
        "fnw": f(inputs["final_norm_w"].reshape(1, D)),
    }
    consts = [_consts(0), _consts(1)]
    maps = []
    for core in range(8):
        b, p = core // 2, core % 2
        m = dict(shared)
        m.update(consts[p])
        m["xn"] = x[b]
        m["xo"] = np.ascontiguousarray(x[b][_own_rows(p)])
        m["cT"] = np.ascontiguousarray(cvec[b].reshape(8, 128).T)
        maps.append(m)
    return maps


_NC_CACHE = {}


def kernel(**inputs):
    if "nc" not in _NC_CACHE:
        _NC_CACHE["nc"] = build_nc()
    nc = _NC_CACHE["nc"]
    maps = make_in_maps(inputs)
    res = run_bass_kernel_spmd(nc, maps, core_ids=list(range(8)))
    out = np.empty((4, S, D), np.float32)
    for core in range(8):
        b, p = core // 2, core % 2
        out[b][_own_rows(p)] = res.results[core]["out"]
    return out
```
